# Optimizing a Trainium2 kernel written in Bass

```python
import math
import jax, jax.numpy as jnp
from jax import lax
import numpy as np

D_MODEL = 1024
BATCH = 32
SEQ = 2048
DEPTH = 2

HEAD_DIM = 64
A_HEADS = 8
IDX_HEADS = 4
IDX_DIM = 64
TOPK_MAX = 256
B_HEADS = 4
B_VDIM = 2 * HEAD_DIM
D_A = A_HEADS * HEAD_DIM
D_B = B_HEADS * B_VDIM
FFN_HIDDEN = -(-8 * D_MODEL // (3 * 256)) * 256
ROPE_THETA = 10000.0
EPS = 1e-6
DENSE_Q_BLOCK = 128
SPARSE_Q_BLOCK = 32
POS_OFFSET_MAX = 4096
IN_WIDTHS = (D_A, D_A, D_A,
             IDX_HEADS * IDX_DIM, IDX_DIM, IDX_HEADS,
             B_HEADS * 2 * HEAD_DIM, B_HEADS * 2 * HEAD_DIM, D_B,
             2 * D_MODEL)
D_IN = sum(IN_WIDTHS)

kernel_name = "hybrid_dsa_diffattn_gated_swiglu"


def rms_norm(x, g):
    x32 = x.astype(jnp.float32)
    y = x32 * lax.rsqrt(jnp.mean(x32 * x32, axis=-1, keepdims=True) + EPS)
    return (y * g.astype(jnp.float32)).astype(x.dtype)


def rope_tables(positions):
    inv_freq = 1.0 / (ROPE_THETA ** (jnp.arange(0, HEAD_DIM, 2, dtype=jnp.float32) / HEAD_DIM))
    ang = positions.astype(jnp.float32)[..., None] * inv_freq
    return jnp.cos(ang)[:, :, None, :], jnp.sin(ang)[:, :, None, :]


def apply_rope(x, cos, sin):
    x32 = x.astype(jnp.float32)
    x1, x2 = jnp.split(x32, 2, axis=-1)
    out = jnp.concatenate([x1 * cos - x2 * sin, x2 * cos + x1 * sin], axis=-1)
    return out.astype(x.dtype)


def split_cols(proj):
    offs = np.cumsum(np.array(IN_WIDTHS))[:-1].tolist()
    return jnp.split(proj, offs, axis=-1)


def sparse_attention(q, k, v, q_idx, k_idx, w_idx, k_sel):
    B, S, H, dh = q.shape
    n_blk = S // SPARSE_Q_BLOCK
    key_pos = jnp.arange(S)
    k_idx32 = k_idx.astype(jnp.float32)

    def block(i):
        start = i * SPARSE_Q_BLOCK
        qb = lax.dynamic_slice_in_dim(q, start, SPARSE_Q_BLOCK, axis=1)
        qib = lax.dynamic_slice_in_dim(q_idx, start, SPARSE_Q_BLOCK, axis=1)
        wb = lax.dynamic_slice_in_dim(w_idx, start, SPARSE_Q_BLOCK, axis=1)
        q_pos = start + jnp.arange(SPARSE_Q_BLOCK)
        causal = key_pos[None, :] <= q_pos[:, None]
        dots = jnp.einsum('bqhd,bsd->bqhs', qib.astype(jnp.float32), k_idx32) * (IDX_DIM ** -0.5)
        score = jnp.einsum('bqh,bqhs->bqs', wb.astype(jnp.float32), jax.nn.relu(dots))
        score = jnp.where(causal[None], score, -jnp.inf)
        _, top_idx = lax.top_k(score, k_sel)
        valid = top_idx <= q_pos[None, :, None]
        kg = jax.vmap(lambda kb, ib: kb[ib])(k, top_idx)
        vg = jax.vmap(lambda vb, ib: vb[ib])(v, top_idx)
        logits = jnp.einsum('bqhd,bqkhd->bqhk', qb.astype(jnp.float32),
                            kg.astype(jnp.float32)) * (dh ** -0.5)
        logits = jnp.where(valid[:, :, None, :], logits, -jnp.inf)
        p = jax.nn.softmax(logits, axis=-1).astype(v.dtype)
        return jnp.einsum('bqhk,bqkhd->bqhd', p, vg)

    out = lax.map(block, jnp.arange(n_blk))
    return jnp.moveaxis(out, 0, 1).reshape(B, S, H, dh)


def diff_attention(q, k, v, lam):
    B, S, H, _, dh = q.shape
    n_blk = S // DENSE_Q_BLOCK
    key_pos = jnp.arange(S)
    k32 = k.astype(jnp.float32)

    def block(i):
        start = i * DENSE_Q_BLOCK
        qb = lax.dynamic_slice_in_dim(q, start, DENSE_Q_BLOCK, axis=1)
        q_pos = start + jnp.arange(DENSE_Q_BLOCK)
        causal = key_pos[None, :] <= q_pos[:, None]
        logits = jnp.einsum('bqhcd,bshcd->bhcqs', qb.astype(jnp.float32), k32) * (dh ** -0.5)
        logits = jnp.where(causal[None, None, None], logits, -jnp.inf)
        p = jax.nn.softmax(logits, axis=-1)
        attn = (p[:, :, 0] - lam * p[:, :, 1]).astype(v.dtype)
        return jnp.einsum('bhqs,bshe->bqhe', attn, v)

    out = lax.map(block, jnp.arange(n_blk))
    return jnp.moveaxis(out, 0, 1).reshape(B, S, H, 2 * dh)


def hybrid_layer(x, cos, sin, layer_idx, attn_norm, w_in, gate_bias, a_q_norm, a_k_norm,
                 idx_k_norm, b_q_norm, b_k_norm, diff_lambda, b_subln, w_up_a, w_up_b,
                 w_out, ffn_norm, w_ffn_in, w_ffn_out):
    B, S, _ = x.shape
    k_sel = min(TOPK_MAX, S // 4)
    h = rms_norm(x, attn_norm)
    proj = h @ w_in
    aq, ak, av, iq, ik, iw, bq, bk, bv, gates = split_cols(proj)

    aq = apply_rope(rms_norm(aq.reshape(B, S, A_HEADS, HEAD_DIM), a_q_norm), cos, sin)
    ak = apply_rope(rms_norm(ak.reshape(B, S, A_HEADS, HEAD_DIM), a_k_norm), cos, sin)
    av = av.reshape(B, S, A_HEADS, HEAD_DIM)
    iq = apply_rope(iq.reshape(B, S, IDX_HEADS, IDX_DIM), cos, sin)
    ik = apply_rope(rms_norm(ik, idx_k_norm)[:, :, None, :], cos, sin)[:, :, 0, :]
    iw = iw * (IDX_HEADS ** -0.5)
    ya = sparse_attention(aq, ak, av, iq, ik, iw, k_sel).reshape(B, S, D_A)

    bq = apply_rope(rms_norm(bq.reshape(B, S, 2 * B_HEADS, HEAD_DIM), b_q_norm), cos, sin)
    bk = apply_rope(rms_norm(bk.reshape(B, S, 2 * B_HEADS, HEAD_DIM), b_k_norm), cos, sin)
    bq = bq.reshape(B, S, B_HEADS, 2, HEAD_DIM)
    bk = bk.reshape(B, S, B_HEADS, 2, HEAD_DIM)
    bv = bv.reshape(B, S, B_HEADS, B_VDIM)
    lam_init = 0.8 - 0.6 * math.exp(-0.3 * layer_idx)
    dl = diff_lambda.astype(jnp.float32)
    lam = jnp.exp(jnp.sum(dl[0] * dl[1])) - jnp.exp(jnp.sum(dl[2] * dl[3])) + lam_init
    yb = diff_attention(bq, bk, bv, lam)
    yb = (rms_norm(yb, b_subln) * (1.0 - lam_init)).reshape(B, S, D_B)

    g = jax.nn.sigmoid((gates + gate_bias).astype(jnp.float32)).astype(x.dtype)
    g_a, g_b = jnp.split(g, 2, axis=-1)
    merged = g_a * (ya @ w_up_a) + g_b * (yb @ w_up_b)
    x = x + merged @ w_out

    h = rms_norm(x, ffn_norm)
    gate, up = jnp.split(h @ w_ffn_in, 2, axis=-1)
    return x + (jax.nn.silu(gate) * up) @ w_ffn_out


def setup_inputs(seed: int = 0) -> dict:
    key = jax.random.key(seed)
    ks = jax.random.split(key, 20)
    f32 = jnp.float32

    def dense(k, shape, fan_in):
        return jax.random.normal(k, shape, f32) * (fan_in ** -0.5)

    def gain(k, shape):
        return 1.0 + 0.02 * jax.random.normal(k, shape, f32)

    x = jax.random.normal(ks[0], (BATCH, SEQ, D_MODEL), f32)
    offset = jax.random.randint(ks[1], (BATCH, 1), 0, POS_OFFSET_MAX, dtype=jnp.int32)
    positions = (offset + jnp.arange(SEQ, dtype=jnp.int32)[None, :]).astype(jnp.int32)
    return {
        "x": x,
        "positions": positions,
        "attn_norm": gain(ks[2], (DEPTH, D_MODEL)),
        "w_in": dense(ks[3], (DEPTH, D_MODEL, D_IN), D_MODEL),
        "gate_bias": 0.1 * jax.random.normal(ks[4], (DEPTH, 2 * D_MODEL), f32),
        "a_q_norm": gain(ks[5], (DEPTH, HEAD_DIM)),
        "a_k_norm": gain(ks[6], (DEPTH, HEAD_DIM)),
        "idx_k_norm": gain(ks[7], (DEPTH, IDX_DIM)),
        "b_q_norm": gain(ks[8], (DEPTH, HEAD_DIM)),
        "b_k_norm": gain(ks[9], (DEPTH, HEAD_DIM)),
        "diff_lambda": 0.1 * jax.random.normal(ks[10], (DEPTH, 4, HEAD_DIM), f32),
        "b_subln": gain(ks[11], (DEPTH, B_VDIM)),
        "w_up_a": dense(ks[12], (DEPTH, D_A, D_MODEL), D_A),
        "w_up_b": dense(ks[13], (DEPTH, D_B, D_MODEL), D_B),
        "w_out": dense(ks[14], (DEPTH, D_MODEL, D_MODEL), D_MODEL),
        "ffn_norm": gain(ks[15], (DEPTH, D_MODEL)),
        "w_ffn_in": dense(ks[16], (DEPTH, D_MODEL, 2 * FFN_HIDDEN), D_MODEL),
        "w_ffn_out": dense(ks[17], (DEPTH, FFN_HIDDEN, D_MODEL), FFN_HIDDEN),
    }


def reference(x, positions, attn_norm, w_in, gate_bias, a_q_norm, a_k_norm, idx_k_norm,
              b_q_norm, b_k_norm, diff_lambda, b_subln, w_up_a, w_up_b, w_out,
              ffn_norm, w_ffn_in, w_ffn_out):
    cos, sin = rope_tables(positions)
    for l in range(DEPTH):
        x = hybrid_layer(x, cos, sin, l, attn_norm[l], w_in[l], gate_bias[l], a_q_norm[l],
                         a_k_norm[l], idx_k_norm[l], b_q_norm[l], b_k_norm[l],
                         diff_lambda[l], b_subln[l], w_up_a[l], w_up_b[l], w_out[l],
                         ffn_norm[l], w_ffn_in[l], w_ffn_out[l])
    return x
```

```python
import contextlib
import math
import numpy as np
import concourse.bass as bass
import concourse.mybir as mybir
from concourse.bass_utils import run_bass_kernel_spmd

dt = mybir.dt
F32, BF16, I32, U8 = dt.float32, dt.bfloat16, dt.int32, dt.uint8
ALU, AF, AX = mybir.AluOpType, mybir.ActivationFunctionType, mybir.AxisListType

S = 2048
D = 1024
DIN = 5444
FH = 2816
NT = 16
EPS = 1e-6
NBIS = 16
ACT_COUNT = True
EPOCH = 6000
ENGS = ("pe", "dve", "act", "pool", "sp")
ENGMAP = {"pe": "tensor", "dve": "vector", "act": "scalar", "pool": "gpsimd", "sp": "sync"}
O_AQ, O_AK, O_AV, O_IQ, O_IK, O_IW, O_BQ, O_BK, O_BV, O_G = 0, 512, 1024, 1536, 1792, 1856, 1860, 2372, 2884, 3396


class Op:
    __slots__ = ("eng", "fn", "deps", "sem", "val", "is_dma")

    def __init__(self, eng, fn, is_dma=False):
        self.eng, self.fn, self.is_dma = eng, fn, is_dma
        self.deps, self.sem, self.val = [], None, None


class Prog:
    def __init__(self, nc, stack):
        self.nc, self.stack = nc, stack
        self.pending = {e: [] for e in ENGS}
        self.cnt = {e: 0 for e in ENGS}
        self.last_op = {e: None for e in ENGS}
        self.dmas = []
        self.slot_cnt = {}
        self.sems = {}
        self.waited = {e: {} for e in ENGS}
        self.last_w, self.readers = {}, {}
        self.final = []

    def _sem(self, name):
        if name not in self.sems:
            self.sems[name] = self.stack.enter_context(self.nc.semaphore(name))
        return self.sems[name]

    def op(self, eng, fn, reads=(), writes=(), dma=None):
        o = Op(eng, fn, is_dma=dma is not None)
        deps = set()
        for r in reads:
            w = self.last_w.get(r)
            if w is not None:
                deps.add(w)
        for k in writes:
            w = self.last_w.get(k)
            if w is not None and (w.eng != eng or w.is_dma or o.is_dma or eng != "pe"):
                deps.add(w)
            for rd in self.readers.get(k, ()):
                if rd.eng != eng or rd.is_dma or o.is_dma or eng != "pe":
                    deps.add(rd)
        deps.discard(o)
        o.deps = list(deps)
        for r in reads:
            self.readers.setdefault(r, []).append(o)
        for k in writes:
            self.last_w[k] = o
            self.readers[k] = []
        if o.is_dma:
            c = self.slot_cnt.get(dma, 0)
            ep, c2 = c // EPOCH, c % EPOCH + 16
            if c2 > EPOCH:
                ep, c2 = ep + 1, 16
                c = ep * EPOCH
            self.slot_cnt[dma] = ep * EPOCH + c2
            o.sem, o.val = self._sem("d%s_%d" % (dma, ep)), c2
            self.dmas.append(o)
        else:
            c = self.cnt[eng]
            o.sem, o.val = self._sem("e%s_%d" % (eng, c // EPOCH)), c % EPOCH + 1
            self.cnt[eng] = c + 1
            self.last_op[eng] = o
        self.pending[eng].append(o)
        return o

    def barrier(self):
        lasts = [self.last_op[e] for e in ENGS if self.last_op[e] is not None] + list(self.dmas)
        for e in ENGS:
            o = Op(e, None)
            o.deps = lasts
            self.pending[e].append(o)
        self.dmas = []
        self.last_w, self.readers = {}, {}

    def emit(self, final=False):
        with self.nc.Block() as block:
            for e in ENGS:
                ops = self.pending[e]
                if not ops and not (final and e == "sp"):
                    continue

                def body(eng, ops=ops, e=e):
                    waited = self.waited[e]
                    for o in ops:
                        need = {}
                        for d in o.deps:
                            k = id(d.sem)
                            if waited.get(k, 0) >= d.val:
                                continue
                            if k not in need or need[k][1] < d.val:
                                need[k] = (d.sem, d.val)
                        for k, (s, v) in need.items():
                            eng.wait_ge(s, v)
                            waited[k] = v
                        if o.fn is None:
                            continue
                        ins = o.fn(eng)
                        ins.then_inc(o.sem, 16 if o.is_dma else 1)
                    if final and e == "sp":
                        for o in self.final:
                            eng.wait_ge(o.sem, o.val)

                getattr(block, ENGMAP[e])(body)
        self.pending = {e: [] for e in ENGS}


_uid = [0]


def uname(n):
    _uid[0] += 1
    return "%s_%d" % (n, _uid[0])


def build(n_seq, depth, dbg=False):
    nc = bass.Bass("TRN2", target_bir_lowering=False, dynamic_dma_scratch_size=4096)
    dram = lambda n, s, d, k="ExternalInput": nc.dram_tensor(n, s, d, kind=k).ap()
    x_d = dram("x", [n_seq, S, D], F32)
    pos_d = dram("pos", [n_seq, 128, NT], I32)
    win_d = dram("w_in", [depth, D, DIN], F32)
    wua_d = dram("w_up_a", [depth, 512, D], F32)
    wub_d = dram("w_up_b", [depth, 512, D], F32)
    wo_d = dram("w_out", [depth, D, D], F32)
    wfi_d = dram("w_ffn_in", [depth, D, 2 * FH], F32)
    wfo_d = dram("w_ffn_out", [depth, FH, D], F32)
    gattn_d = dram("gattn", [128, depth, 8], F32)
    gffn_d = dram("gffn", [128, depth, 8], F32)
    gbias_d = dram("gbias", [128, depth, 16], F32)
    hn_d = dram("hnorm", [128, depth, 5, 64], F32)
    subln_d = dram("subln", [128, depth], F32)
    dlam_d = dram("dlam", [128, depth, 4, 64], F32)
    cf_d = dram("cf32", [128, 128 * 3 + 32 + NBIS + 1], F32)
    out_d = dram("out", [n_seq, S, D], F32, "ExternalOutput")

    with contextlib.ExitStack() as st:
        sb = lambda n, s, d: st.enter_context(nc.sbuf_tensor(uname(n), s, d))
        P = Prog(nc, st)

        def dump(name, src, shape, dty, keys):
            if not dbg:
                return
            dd = nc.dram_tensor(name, shape, dty, kind="ExternalOutput").ap()
            o = P.op("sp", lambda e: e.dma_start(out=dd, in_=src), reads=keys, dma="dbg")
            P.final.append(o)
        xT = sb("xT", [128, 8, S], F32)
        cf = sb("cf", [128, 128 * 3 + 32 + NBIS + 1], F32)
        ident_f, caus_b, invf = cf[:, 0:128], cf[:, 128:256], cf[:, 384:416]
        pow2 = cf[:, 416:416 + NBIS + 1]
        ident_b = sb("ident_b", [128, 128], BF16)
        triT_b = sb("triT_b", [128, 128], BF16)
        ones_b = sb("ones_b", [128, 128], BF16)
        gattn = sb("gattn_s", [128, depth, 8], F32)
        gffn = sb("gffn_s", [128, depth, 8], F32)
        gbias = sb("gbias_s", [128, depth, 16], F32)
        hn = sb("hn_s", [128, depth, 5, 64], F32)
        subln = sb("subln_s", [128, depth], F32)
        lam = sb("lam_s", [128, depth], F32)
        nlam = sb("nlam_s", [128, depth], F32)
        cosT = sb("cosT", [128, NT, 32], F32)
        sinT = sb("sinT", [128, NT, 32], F32)
        PS = [st.enter_context(nc.psum_tensor("ps%d" % i, [128, 512], F32)) for i in range(6)]
        PT = [st.enter_context(nc.psum_tensor("pt%d" % i, [128, 1024], BF16)) for i in range(2)]
        psk = lambda i: "ps%d" % i
        ptk = lambda i: "pt%d" % i

        cst = contextlib.ExitStack()
        dlam = cst.enter_context(nc.sbuf_tensor(uname("dlam_s"), [128, depth, 4, 64], F32))
        small = cst.enter_context(nc.sbuf_tensor(uname("small"), [128, 80], F32))
        P.op("sp", lambda e: e.dma_start(out=cf[:], in_=cf_d[:, :]), writes=["cf"], dma="c0")
        for nm, t, d_ in (("gattn", gattn, gattn_d), ("gffn", gffn, gffn_d), ("gbias", gbias, gbias_d),
                          ("hn", hn, hn_d), ("subln", subln, subln_d), ("dlam", dlam, dlam_d)):
            P.op("sp", lambda e, t=t, d_=d_: e.dma_start(out=t[:], in_=d_), writes=[nm], dma="c_" + nm)
        P.op("dve", lambda e: e.tensor_copy(out=ident_b[:], in_=cf[:, 0:128]), reads=["cf"], writes=["ident_b"])
        P.op("dve", lambda e: e.tensor_copy(out=triT_b[:], in_=cf[:, 256:384]), reads=["cf"], writes=["triT_b"])
        P.op("dve", lambda e: e.memset(ones_b[:], 1.0), writes=["ones_b"])
        for l in range(depth):
            lam_init = 0.8 - 0.6 * math.exp(-0.3 * l)
            P.op("dve", lambda e, l=l: e.tensor_tensor(out=small[:, 0:64], in0=dlam[:, l, 0, :], in1=dlam[:, l, 1, :], op=ALU.mult),
                 reads=["dlam"], writes=["sm0"])
            P.op("dve", lambda e: e.tensor_reduce(out=small[:, 64:65], in_=small[:, 0:64], axis=AX.X, op=ALU.add),
                 reads=["sm0"], writes=["sm0"])
            P.op("act", lambda e: e.activation(out=small[:, 65:66], in_=small[:, 64:65], func=AF.Exp), reads=["sm0"], writes=["sm1"])
            P.op("dve", lambda e, l=l: e.tensor_tensor(out=small[:, 0:64], in0=dlam[:, l, 2, :], in1=dlam[:, l, 3, :], op=ALU.mult),
                 reads=["dlam", "sm1"], writes=["sm0"])
            P.op("dve", lambda e: e.tensor_reduce(out=small[:, 64:65], in_=small[:, 0:64], axis=AX.X, op=ALU.add),
                 reads=["sm0"], writes=["sm0"])
            P.op("act", lambda e: e.activation(out=small[:, 66:67], in_=small[:, 64:65], func=AF.Exp), reads=["sm0"], writes=["sm2"])
            P.op("dve", lambda e, l=l, li=lam_init: e.scalar_tensor_tensor(out=lam[:, l:l + 1], in0=small[:, 65:66], scalar=li,
                                                                          in1=small[:, 66:67], op0=ALU.add, op1=ALU.subtract),
                 reads=["sm1", "sm2"], writes=["lam"])
            P.op("dve", lambda e, l=l: e.tensor_scalar(out=nlam[:, l:l + 1], in0=lam[:, l:l + 1], scalar1=-1.0, scalar2=None, op0=ALU.mult),
                 reads=["lam"], writes=["nlam"])
        P.barrier()
        P.emit()
        cst.close()

        def wload(dst, src, key, slot):
            return P.op("pool", lambda e: e.dma_start(out=dst, in_=src), writes=[key], dma=slot)

        def win_view(l, c0, n):
            return win_d[l].rearrange("(kc p) n -> p kc n", p=128)[:, :, c0:c0 + n]

        def norm_block(g, l, t0, n, dst, dkey, sq, rs, tag, nb=5):
            for c in range(8):
                j = c % 2
                P.op("act", lambda e, c=c, j=j: e.activation(out=sq[:, j, 0:n], in_=xT[:, c, t0:t0 + n], func=AF.Square),
                     reads=["xT"], writes=[tag + "sq%d" % j])
                P.op("pe", lambda e, c=c, j=j: e.matmul(PS[nb][:, 0:n], lhsT=ones_b[:], rhs=sq[:, j, 0:n], start=(c == 0), stop=(c == 7)),
                     reads=[tag + "sq%d" % j, "ones_b"], writes=[psk(nb)])
            P.op("act", lambda e: e.activation(out=rs[:, 0:n], in_=PS[nb][:, 0:n], func=AF.Ln, scale=1.0 / D, bias=eps_t[:, 0:1]),
                 reads=[psk(nb), "eps"], writes=[tag + "rs"])
            P.op("act", lambda e: e.activation(out=rs[:, 0:n], in_=rs[:, 0:n], func=AF.Exp, scale=-0.5), reads=[tag + "rs"], writes=[tag + "rs"])
            for c in range(8):
                P.op("dve", lambda e, c=c: e.scalar_tensor_tensor(out=dst[:, c, 0:n], in0=xT[:, c, t0:t0 + n], scalar=g[:, l, c:c + 1],
                                                                   in1=rs[:, 0:n], op0=ALU.mult, op1=ALU.mult),
                     reads=["xT", tag + "rs", "gains"], writes=[dkey])

        eps_t = sb("eps_t", [128, 1], F32)
        P.op("dve", lambda e: e.memset(eps_t[:], EPS), writes=["eps"])

        def proj_tok(hT, hkey, tt, W, wkey, ncols, bank):
            for kc in range(8):
                P.op("pe", lambda e, kc=kc: e.matmul(PS[bank][:, 0:ncols], lhsT=hT[:, kc, tt * 128:(tt + 1) * 128], rhs=W[:, kc, 0:ncols],
                                                     start=(kc == 0), stop=(kc == 7)),
                     reads=[hkey, wkey], writes=[psk(bank)])

        def postproc(src, skey, H, gain, tile, out, okey, scr, do_norm=True, sfx=""):
            t_sq, t_n, t_a, t_b, ss = scr
            W = H * 64
            ksq, kn, ka, kb2, kss = "t_sq" + sfx, "t_n" + sfx, "t_a" + sfx, "t_b" + sfx, "ss" + sfx
            v3 = lambda ap: ap.rearrange("p (h d) -> p h d", d=64)
            if do_norm:
                P.op("act", lambda e: e.activation(out=t_sq[:, 0:W], in_=src, func=AF.Square), reads=[skey], writes=[ksq])
                P.op("dve", lambda e: e.tensor_reduce(out=ss[:, 0:H], in_=v3(t_sq[:, 0:W]), axis=AX.X, op=ALU.add),
                     reads=[ksq], writes=[kss])
                P.op("act", lambda e: e.activation(out=ss[:, 0:H], in_=ss[:, 0:H], func=AF.Ln, scale=1.0 / 64, bias=eps_t[:, 0:1]), reads=[kss, "eps"], writes=[kss])
                P.op("act", lambda e: e.activation(out=ss[:, 0:H], in_=ss[:, 0:H], func=AF.Exp, scale=-0.5), reads=[kss], writes=[kss])
                P.op("dve", lambda e: e.tensor_tensor(out=v3(t_n[:, 0:W]), in0=v3(src), in1=ss[:, 0:H].unsqueeze(2).to_broadcast([128, H, 64]), op=ALU.mult),
                     reads=[skey, kss], writes=[kn])
                P.op("dve", lambda e: e.tensor_tensor(out=v3(t_n[:, 0:W]), in0=v3(t_n[:, 0:W]), in1=gain.unsqueeze(1).to_broadcast([128, H, 64]), op=ALU.mult),
                     reads=[kn, "gains"], writes=[kn])
            else:
                P.op("act", lambda e: e.activation(out=t_n[:, 0:W], in_=src, func=AF.Copy), reads=[skey], writes=[kn])
            x1, x2 = v3(t_n[:, 0:W])[:, :, 0:32], v3(t_n[:, 0:W])[:, :, 32:64]
            o1, o2 = v3(out)[:, :, 0:32], v3(out)[:, :, 32:64]
            a3 = t_a[:, 0:H * 32].rearrange("p (h d) -> p h d", d=32)
            b3 = t_b[:, 0:H * 32].rearrange("p (h d) -> p h d", d=32)
            a4 = t_a[:, 256:256 + H * 32].rearrange("p (h d) -> p h d", d=32)
            b4 = t_b[:, 256:256 + H * 32].rearrange("p (h d) -> p h d", d=32)
            cs = cosT[:, tile, :].unsqueeze(1).to_broadcast([128, H, 32])
            sn = sinT[:, tile, :].unsqueeze(1).to_broadcast([128, H, 32])
            P.op("pool", lambda e: e.tensor_tensor(out=a3, in0=x1, in1=cs, op=ALU.mult), reads=[kn, "tab"], writes=[ka])
            P.op("pool", lambda e: e.tensor_tensor(out=b3, in0=x2, in1=sn, op=ALU.mult), reads=[kn, "tab"], writes=[kb2])
            P.op("pool", lambda e: e.tensor_tensor(out=a4, in0=x2, in1=cs, op=ALU.mult), reads=[kn, "tab"], writes=[ka + "2"])
            P.op("pool", lambda e: e.tensor_tensor(out=b4, in0=x1, in1=sn, op=ALU.mult), reads=[kn, "tab"], writes=[kb2 + "2"])
            P.op("pool", lambda e: e.tensor_tensor(out=o1, in0=a3, in1=b3, op=ALU.subtract), reads=[ka, kb2], writes=[okey])
            P.op("pool", lambda e: e.tensor_tensor(out=o2, in0=a4, in1=b4, op=ALU.add), reads=[ka + "2", kb2 + "2"], writes=[okey])

        def range_sin(src, shift, dst, ki, kf, r, m):
            twopi = 2.0 * math.pi
            C1, C2 = 6.28125, twopi - 6.28125
            k = "rs_"
            P.op("dve", lambda e: e.tensor_scalar(out=ki, in0=src, scalar1=1.0 / twopi, scalar2=shift / twopi, op0=ALU.mult, op1=ALU.add),
                 reads=["ang"], writes=[k + "ki"])
            P.op("dve", lambda e: e.tensor_copy(out=kf, in_=ki), reads=[k + "ki"], writes=[k + "kf"])
            P.op("dve", lambda e: e.scalar_tensor_tensor(out=r, in0=kf, scalar=-C1, in1=src, op0=ALU.mult, op1=ALU.add),
                 reads=[k + "kf", "ang"], writes=[k + "r"])
            P.op("dve", lambda e: e.scalar_tensor_tensor(out=r, in0=kf, scalar=-C2, in1=r, op0=ALU.mult, op1=ALU.add),
                 reads=[k + "kf", k + "r"], writes=[k + "r"])
            if shift:
                P.op("dve", lambda e: e.tensor_scalar(out=r, in0=r, scalar1=float(shift), scalar2=None, op0=ALU.add), reads=[k + "r"], writes=[k + "r"])
            P.op("dve", lambda e: e.tensor_scalar(out=m, in0=r, scalar1=math.pi, scalar2=twopi, op0=ALU.is_gt, op1=ALU.mult), reads=[k + "r"], writes=[k + "m"])
            P.op("dve", lambda e: e.tensor_tensor(out=r, in0=r, in1=m, op=ALU.subtract), reads=[k + "r", k + "m"], writes=[k + "r"])
            P.op("dve", lambda e: e.tensor_scalar(out=m, in0=r, scalar1=-math.pi, scalar2=twopi, op0=ALU.is_lt, op1=ALU.mult), reads=[k + "r"], writes=[k + "m"])
            P.op("dve", lambda e: e.tensor_tensor(out=r, in0=r, in1=m, op=ALU.add), reads=[k + "r", k + "m"], writes=[k + "r"])
            P.op("dve", lambda e: e.tensor_scalar(out=r, in0=r, scalar1=3.1415925, scalar2=-3.1415925, op0=ALU.min, op1=ALU.max), reads=[k + "r"], writes=[k + "r"])
            P.op("act", lambda e: e.activation(out=dst, in_=r, func=AF.Sin), reads=[k + "r"], writes=["tab"])

        def attention(mixer, l, j, kT, v, qT, maskT, yT, PTt, rD, ysb, sqb, ratio=4):
            nkt = 4 * j + 4
            steps = [(u, kt) for u in range(8) for kt in range(nkt)]
            qs = "qT%d" % (j % 2)

            def geo(i):
                u, kt = steps[i]
                d = kt - 4 * j
                qoff = max(d, 0) * 128
                return u, kt, d, qoff, i % 2, i % 3

            def emit_qk(i):
                u, kt, d, qoff, sbk, ptj = geo(i)
                c, r0 = u // 2, (u % 2) * 64
                if mixer == "A":
                    P.op("pe", lambda e: e.matmul(PS[sbk][:, qoff:512], lhsT=kT[r0:r0 + 64, c, kt * 128:(kt + 1) * 128], rhs=qT[r0:r0 + 64, c, qoff:512], start=True, stop=True),
                         reads=["kT", qs], writes=[psk(sbk)])
                else:
                    P.op("pe", lambda e: e.matmul(PS[sbk][:, qoff:512], lhsT=kT[:, c, kt * 128:(kt + 1) * 128], rhs=qT[:, u % 2, c, qoff:512], start=True, stop=True),
                         reads=["kT", qs], writes=[psk(sbk)])

            emit_qk(0)
            for i in range(len(steps)):
                u, kt, d, qoff, sbk, ptj = geo(i)
                c, r0 = u // 2, (u % 2) * 64
                ob, db = (2, 3) if u % 2 == 0 else (4, 5)
                will_yield = ((i + 1) % ratio == 0)
                if i + 1 < len(steps) and not will_yield:
                    emit_qk(i + 1)
                pk = "PT%d" % ptj
                P.op("act", lambda e, sbk=sbk, ptj=ptj, qoff=qoff: e.activation(out=PTt[:, ptj, qoff:512], in_=PS[sbk][:, qoff:512], func=AF.Exp, scale=0.125),
                     reads=[psk(sbk)], writes=[pk])
                if mixer == "A":
                    P.op("pool", lambda e, ptj=ptj, kt=kt, qoff=qoff: e.tensor_tensor(out=PTt[:, ptj, qoff:512], in0=PTt[:, ptj, qoff:512],
                                                                                    in1=maskT[:, kt, qoff:512], op=ALU.mult),
                         reads=[pk, "maskT%d" % (j % 2)], writes=[pk])
                elif d >= 0:
                    P.op("dve", lambda e, ptj=ptj, qoff=qoff: e.tensor_tensor(out=PTt[:, ptj, qoff:qoff + 128], in0=PTt[:, ptj, qoff:qoff + 128],
                                                                            in1=triT_b[:], op=ALU.mult),
                         reads=[pk, "triT_b"], writes=[pk])
                if mixer == "A":
                    lo = (u - 1) * 64 if u % 2 else u * 64
                    vst = v[:, kt, lo:lo + (128 if u % 2 else 64)]
                    M = 128 if u % 2 else 64
                else:
                    hh = u // 2
                    vst = v[:, kt, hh * 128:(hh + 1) * 128]
                    M = 128
                P.op("pe", lambda e, vst=vst, M=M, ptj=ptj, qoff=qoff, kt=kt, ob=ob: e.matmul(PS[ob][0:M, qoff:512], lhsT=vst, rhs=PTt[:, ptj, qoff:512],
                                                                                              start=(kt == 0), stop=(kt == nkt - 1)),
                     reads=["v", pk], writes=[psk(ob)])
                P.op("pe", lambda e, ptj=ptj, qoff=qoff, kt=kt, db=db: e.matmul(PS[db][:, qoff:512], lhsT=ones_b[:], rhs=PTt[:, ptj, qoff:512],
                                                                                start=(kt == 0), stop=(kt == nkt - 1)),
                     reads=["ones_b", pk], writes=[psk(db)])
                if kt != nkt - 1:
                    if will_yield:
                        yield
                        if i + 1 < len(steps):
                            emit_qk(i + 1)
                    continue
                P.op("act", lambda e, db=db: e.activation(out=rD[:], in_=PS[db][:], func=AF.Ln), reads=[psk(db)], writes=["nrs"])
                P.op("act", lambda e: e.activation(out=rD[:], in_=rD[:], func=AF.Exp, scale=-1.0), reads=["nrs"], writes=["nrs"])
                if mixer == "A":
                    P.op("dve", lambda e, c=c, r0=r0, ob=ob: e.tensor_tensor(out=yT[r0:r0 + 64, c, j * 512:(j + 1) * 512], in0=PS[ob][r0:r0 + 64, :], in1=rD[r0:r0 + 64, :], op=ALU.mult),
                         reads=[psk(ob), "nrs"], writes=["yT"])
                else:
                    hh, comp = u // 2, u % 2
                    if comp == 0:
                        P.op("dve", lambda e, ob=ob: e.tensor_tensor(out=ysb[:, 0, :], in0=PS[ob][:], in1=rD[:], op=ALU.mult), reads=[psk(ob), "nrs"], writes=["ysb0"])
                    else:
                        P.op("dve", lambda e, ob=ob: e.tensor_tensor(out=ysb[:, 1, :], in0=PS[ob][:], in1=rD[:], op=ALU.mult), reads=[psk(ob), "nrs"], writes=["ysb1"])
                        P.op("dve", lambda e: e.scalar_tensor_tensor(out=ysb[:, 0, :], in0=ysb[:, 1, :], scalar=nlam[:, l:l + 1], in1=ysb[:, 0, :], op0=ALU.mult, op1=ALU.add),
                             reads=["ysb0", "ysb1", "nlam"], writes=["ysb0"])
                        P.op("act", lambda e: e.activation(out=sqb[:], in_=ysb[:, 0, :], func=AF.Square), reads=["ysb0"], writes=["sqb"])
                        P.op("pe", lambda e, db=db: e.matmul(PS[db][:, :], lhsT=ones_b[:], rhs=sqb[:], start=True, stop=True), reads=["sqb", "ones_b"], writes=[psk(db)])
                        P.op("act", lambda e, db=db: e.activation(out=rD[:], in_=PS[db][:], func=AF.Ln, scale=1.0 / 128, bias=eps_t[:, 0:1]), reads=[psk(db), "eps"], writes=["nrs"])
                        P.op("act", lambda e: e.activation(out=rD[:], in_=rD[:], func=AF.Exp, scale=-0.5), reads=["nrs"], writes=["nrs"])
                        li = 0.8 - 0.6 * math.exp(-0.3 * l)
                        P.op("dve", lambda e: e.tensor_scalar(out=ysb[:, 1, :], in0=ysb[:, 0, :], scalar1=subln[:, l:l + 1], scalar2=1.0 - li, op0=ALU.mult, op1=ALU.mult),
                             reads=["ysb0", "subln"], writes=["ysb1"])
                        P.op("dve", lambda e, hh=hh: e.tensor_tensor(out=yT[:, hh, j * 512:(j + 1) * 512], in0=ysb[:, 1, :], in1=rD[:], op=ALU.mult),
                             reads=["ysb1", "nrs"], writes=["yT"])
                if will_yield:
                    yield
                    if i + 1 < len(steps):
                        emit_qk(i + 1)

        for b in range(n_seq):
            with contextlib.ExitStack() as ph:
                psb = lambda n, s, d: ph.enter_context(nc.sbuf_tensor(uname(n), s, d))
                pos_i = psb("pos_i", [128, NT], I32)
                pos_f = psb("pos_f", [128, NT], F32)
                ang = psb("ang", [128, NT * 32], F32)
                ki = psb("ki", [128, NT * 32], I32)
                kf = psb("kf", [128, NT * 32], F32)
                rr = psb("rr", [128, NT * 32], F32)
                mm_ = psb("mm_", [128, NT * 32], F32)
                xs = psb("xs", [128, 2, D], F32)
                P.op("sp", lambda e: e.dma_start(out=pos_i[:], in_=pos_d[b]), writes=["pos_i"], dma="pos")
                P.op("dve", lambda e: e.tensor_copy(out=pos_f[:], in_=pos_i[:]), reads=["pos_i"], writes=["pos_f"])
                for t in range(NT):
                    P.op("dve", lambda e, t=t: e.tensor_scalar(out=ang[:, t * 32:(t + 1) * 32], in0=invf, scalar1=pos_f[:, t:t + 1], scalar2=None, op0=ALU.mult),
                         reads=["pos_f", "cf"], writes=["ang"])
                range_sin(ang[:], 0.0, sinT[:].rearrange("p t d -> p (t d)"), ki[:], kf[:], rr[:], mm_[:])
                range_sin(ang[:], math.pi / 2, cosT[:].rearrange("p t d -> p (t d)"), ki[:], kf[:], rr[:], mm_[:])
                for t in range(NT):
                    jx = t % 2
                    P.op("sp", lambda e, t=t, jx=jx: e.dma_start(out=xs[:, jx, :], in_=x_d[b, t * 128:(t + 1) * 128, :]), writes=["xs%d" % jx], dma="xs%d" % jx)
                    for half in range(2):
                        bank = (2 * t + half) % 4
                        for q in range(4):
                            c = half * 4 + q
                            P.op("pe", lambda e, jx=jx, c=c, q=q, bank=bank: e.transpose(out=PS[bank][:, q * 128:(q + 1) * 128], in_=xs[:, jx, c * 128:(c + 1) * 128], identity=ident_f),
                                 reads=["xs%d" % jx, "cf"], writes=[psk(bank)])
                        eng = "act" if half == 0 else "dve"
                        src = PS[bank][:, :].rearrange("p (c t) -> p c t", t=128)
                        dst = xT[:, half * 4:half * 4 + 4, t * 128:(t + 1) * 128]
                        if eng == "act":
                            P.op("act", lambda e, src=src, dst=dst: e.activation(out=dst, in_=src, func=AF.Copy), reads=[psk(bank)], writes=["xT"])
                        else:
                            P.op("dve", lambda e, src=src, dst=dst: e.tensor_copy(out=dst, in_=src), reads=[psk(bank)], writes=["xT"])
                P.barrier()
                P.emit()

            for l in range(depth):
                with contextlib.ExitStack() as ph:
                    psb = lambda n, s, d: ph.enter_context(nc.sbuf_tensor(uname(n), s, d))
                    yaT = psb("yaT", [128, 4, S], BF16)
                    ybT = None
                    for mixer in ("A", "B"):
                        if mixer == "B":
                            ybT = psb("ybT", [128, 4, S], BF16)
                        with contextlib.ExitStack() as ph2:
                            psb2 = lambda n, s, d: ph2.enter_context(nc.sbuf_tensor(uname(n), s, d))
                            kT = psb2("kT", [128, 4, S], BF16)
                            v = psb2("v", [128, NT, 512], BF16)
                            ikT = psb2("ikT", [64, S], BF16)
                            hT = psb2("hT", [128, 8, 512], BF16)
                            sq = psb2("sq", [128, 2, 512], BF16)
                            rs = psb2("rs", [128, 512], F32)
                            scrs = []
                            for si in range(2):
                                scrs.append((psb2("t_sq", [128, 512], BF16), psb2("t_n", [128, 512], F32), psb2("t_a", [128, 512], F32),
                                             psb2("t_b", [128, 512], F32), psb2("ss", [128, 8], F32)))
                            qkb = [psb2("qkb", [128, 512], BF16) for _ in range(2)]
                            ikb = [psb2("ikb", [128, 256], BF16) for _ in range(2)]
                            W1 = psb2("W1", [128, 8, 512], BF16)
                            W3 = psb2("W3", [128, 8, 324], BF16) if mixer == "A" else None
                            kvs = contextlib.ExitStack()
                            W2 = kvs.enter_context(nc.sbuf_tensor(uname("W2"), [128, 8, 512], BF16))
                            yT = yaT if mixer == "A" else ybT
                            gk = hn[:, l, 1, :] if mixer == "A" else hn[:, l, 4, :]
                            gq = hn[:, l, 0, :] if mixer == "A" else hn[:, l, 3, :]
                            ok_, ov_, oq_ = (O_AK, O_AV, O_AQ) if mixer == "A" else (O_BK, O_BV, O_BQ)
                            wload(W1[:], win_view(l, ok_, 512), "W1", "W1")
                            wload(W2[:], win_view(l, ov_, 512), "W2", "W2")
                            if mixer == "A":
                                wload(W3[:], win_view(l, O_IQ, 324), "W3", "W3")
                            def kv_transposes(tile, si):
                                sfx = str(si)
                                for c in range(4):
                                    P.op("pe", lambda e, c=c, si=si: e.transpose(out=PT[0][:, c * 128:(c + 1) * 128], in_=qkb[si][:, c * 128:(c + 1) * 128], identity=ident_b[:]),
                                         reads=["qkb" + sfx, "ident_b"], writes=[ptk(0)])
                                P.op("act", lambda e, tile=tile: e.activation(out=kT[:, :, tile * 128:(tile + 1) * 128], in_=PT[0][:, 0:512].rearrange("p (c t) -> p c t", t=128), func=AF.Copy),
                                     reads=[ptk(0)], writes=["kT"])
                                if mixer == "A":
                                    P.op("pe", lambda e, si=si: e.transpose(out=PT[1][0:64, 0:128], in_=ikb[si][:, 0:64], identity=ident_b[:]), reads=["ikb" + sfx, "ident_b"], writes=[ptk(1)])
                                    P.op("dve", lambda e, tile=tile: e.tensor_copy(out=ikT[:, tile * 128:(tile + 1) * 128], in_=PT[1][0:64, 0:128]), reads=[ptk(1)], writes=["ikT"])

                            kv_pending = None
                            for blk in range(4):
                                norm_block(gattn, l, blk * 512, 512, hT, "hT", sq, rs, "n")
                                for tt in range(4):
                                    tile = blk * 4 + tt
                                    si = tile % 2
                                    sfx = str(si)
                                    kb_, vb_ = tile % 2, 2 + tile % 2
                                    proj_tok(hT, "hT", tt, W1, "W1", 512, kb_)
                                    proj_tok(hT, "hT", tt, W2, "W2", 512, vb_)
                                    P.op("act", lambda e, tile=tile, vb_=vb_: e.activation(out=v[:, tile, :], in_=PS[vb_][:, :], func=AF.Copy), reads=[psk(vb_)], writes=["v"])
                                    postproc(PS[kb_][:, :], psk(kb_), 8, gk, tile, qkb[si][:, :], "qkb" + sfx, scrs[si], sfx=sfx)
                                    if mixer == "A":
                                        proj_tok(hT, "hT", tt, W3, "W3", 324, 4)
                                        postproc(PS[4][:, 256:320], psk(4), 1, hn[:, l, 2, :], tile, ikb[si][:, 0:64], "ikb" + sfx, scrs[si], sfx=sfx)
                                    if kv_pending is not None:
                                        kv_transposes(*kv_pending)
                                    kv_pending = (tile, si)
                            kv_transposes(*kv_pending)
                            P.barrier()
                            P.emit()
                            kvs.close()
                            wload(W1[:], win_view(l, oq_, 512), "W1", "W1")
                            if mixer == "A":
                                qT = psb2("qT", [128, 2, 4, 512], BF16)
                            else:
                                qT = psb2("qT", [128, 2, 2, 4, 512], BF16)
                                P.op("pool", lambda e: e.memset(qT[64:128, :, 0, :, :], 0.0), writes=["qT0", "qT1"])
                                P.op("pool", lambda e: e.memset(qT[0:64, :, 1, :, :], 0.0), writes=["qT0", "qT1"])
                            PTt = psb2("PTt", [128, 3, 512], BF16)
                            rD = rs
                            ysb = psb2("ysb", [128, 2, 512], F32) if mixer == "B" else None
                            sqb = psb2("sqb", [128, 512], BF16) if mixer == "B" else None
                            if mixer == "A":
                                iqT = [psb2("iqT", [64, 4, 128], BF16) for _ in range(2)]
                                iw = [psb2("iw", [128, 12], F32) for _ in range(2)]
                                Sc = [psb2("Sc", [128, S], F32) for _ in range(2)]
                                junk = psb2("junk", [128, S], U8)
                                rl = psb2("rl", [128, 512], F32)
                                mk = psb2("mk", [128, 2, 512], BF16)
                                maskT = psb2("maskT", [128, 2, NT, 512], U8)
                                bis = [psb2("bis", [128, NBIS + 8], F32) for _ in range(2)]
                            else:
                                maskT = None

                            def stage1(j):
                                par = j % 2
                                pend_masks = []
                                norm_block(gattn, l, j * 512, 512, hT, "hT", sq, rs, "n", nb=1)
                                yield
                                for tt in range(4):
                                    qi = 4 * j + tt
                                    si = tt % 2
                                    sfx = str(si)
                                    proj_tok(hT, "hT", tt, W1, "W1", 512, 0)
                                    postproc(PS[0][:, :], psk(0), 8, gq, qi, qkb[si][:, :], "qkb" + sfx, scrs[si], sfx=sfx)
                                    nk = 128 * (qi + 1)
                                    if mixer == "A":
                                        Sct, kS, bt, iwt, iqt = Sc[si], "Sc" + sfx, bis[si], iw[si], iqT[si]
                                        thr = bt[:, NBIS + 4:NBIS + 5]
                                        mn, mx, w0 = bt[:, NBIS + 1:NBIS + 2], bt[:, NBIS + 2:NBIS + 3], bt[:, NBIS + 3:NBIS + 4]
                                        mid = bt[:, NBIS + 5:NBIS + 6]
                                        if qi >= 2:
                                            proj_tok(hT, "hT", tt, W3, "W3", 324, 1)
                                            P.op("act", lambda e, iwt=iwt: e.activation(out=iwt[:, 0:4], in_=PS[1][:, 320:324], func=AF.Copy), reads=[psk(1)], writes=["iw" + sfx])
                                            P.op("act", lambda e, iwt=iwt: e.activation(out=iwt[:, 4:8], in_=PS[1][:, 320:324], func=AF.Abs), reads=[psk(1)], writes=["iwa" + sfx])
                                            P.op("dve", lambda e, iwt=iwt: e.tensor_scalar(out=iwt[:, 8:12], in0=iwt[:, 0:4], scalar1=0.0, scalar2=2.0, op0=ALU.is_ge, op1=ALU.mult), reads=["iw" + sfx], writes=["iws" + sfx])
                                            P.op("dve", lambda e, iwt=iwt: e.tensor_scalar(out=iwt[:, 8:12], in0=iwt[:, 8:12], scalar1=-1.0, scalar2=None, op0=ALU.add), reads=["iws" + sfx], writes=["iws" + sfx])
                                            postproc(PS[1][:, 0:256], psk(1), 4, None, qi, ikb[si][:, 0:256], "ikb" + sfx, scrs[si], do_norm=False, sfx=sfx)
                                    yield
                                    yield
                                    for c in range(4):
                                        P.op("pe", lambda e, c=c, si=si: e.transpose(out=PT[0][:, c * 128:(c + 1) * 128], in_=qkb[si][:, c * 128:(c + 1) * 128], identity=ident_b[:]),
                                             reads=["qkb" + sfx, "ident_b"], writes=[ptk(0)])
                                    if mixer == "A":
                                        P.op("act", lambda e, tt=tt, par=par: e.activation(out=qT[:, par, :, tt * 128:(tt + 1) * 128], in_=PT[0][:, 0:512].rearrange("p (c t) -> p c t", t=128), func=AF.Copy),
                                             reads=[ptk(0)], writes=["qT%d" % par])
                                    else:
                                        P.op("act", lambda e, tt=tt, par=par: e.activation(out=qT[0:64, par, 0, :, tt * 128:(tt + 1) * 128], in_=PT[0][0:64, 0:512].rearrange("p (c t) -> p c t", t=128), func=AF.Copy),
                                             reads=[ptk(0)], writes=["qT%d" % par])
                                        P.op("act", lambda e, tt=tt, par=par: e.activation(out=qT[64:128, par, 1, :, tt * 128:(tt + 1) * 128], in_=PT[0][64:128, 0:512].rearrange("p (c t) -> p c t", t=128), func=AF.Copy),
                                             reads=[ptk(0)], writes=["qT%d" % par])
                                    yield
                                    if mixer != "A":
                                        continue
                                    if tt == 2 and pend_masks:
                                        yield from pend_masks.pop()
                                    if qi < 2:
                                        P.op("dve", lambda e, nk=nk, Sct=Sct: e.memset(Sct[:, 0:nk], 0.0), writes=[kS])
                                        P.op("dve", lambda e, qi=qi, Sct=Sct: e.tensor_copy(out=Sct[:, qi * 128:(qi + 1) * 128], in_=caus_b), reads=["cf"], writes=[kS])
                                        P.op("dve", lambda e, thr=thr: e.memset(thr, -1e29), writes=["thr" + sfx])
                                    else:
                                        for h in range(4):
                                            P.op("pe", lambda e, h=h, si=si: e.transpose(out=PT[1][0:64, h * 128:(h + 1) * 128], in_=ikb[si][:, h * 64:(h + 1) * 64], identity=ident_b[:]),
                                                 reads=["ikb" + sfx, "ident_b"], writes=[ptk(1)])
                                        P.op("dve", lambda e, iqt=iqt: e.tensor_copy(out=iqt[:, :, :], in_=PT[1][0:64, 0:512].rearrange("p (h t) -> p h t", t=128)), reads=[ptk(1)], writes=["iqT" + sfx])
                                        yield
                                        for kb in range((nk + 511) // 512):
                                            w = min(512, nk - kb * 512)
                                            for h in range(4):
                                                bank = h % 2
                                                P.op("pe", lambda e, h=h, kb=kb, w=w, bank=bank, iqt=iqt: e.matmul(PS[bank][:, 0:w], lhsT=iqt[:, h, :], rhs=ikT[:, kb * 512:kb * 512 + w], start=True, stop=True),
                                                     reads=["iqT" + sfx, "ikT"], writes=[psk(bank)])
                                                P.op("act", lambda e, h=h, w=w, bank=bank, iwt=iwt: e.activation(out=rl[:, 0:w], in_=PS[bank][:, 0:w], func=AF.Relu, scale=iwt[:, 4 + h:5 + h]),
                                                     reads=[psk(bank), "iwa" + sfx], writes=["rl"])
                                                if h == 0:
                                                    P.op("dve", lambda e, kb=kb, w=w, Sct=Sct, iwt=iwt: e.tensor_scalar(out=Sct[:, kb * 512:kb * 512 + w], in0=rl[:, 0:w], scalar1=iwt[:, 8:9], scalar2=None, op0=ALU.mult),
                                                         reads=["rl", "iws" + sfx], writes=[kS])
                                                else:
                                                    P.op("dve", lambda e, h=h, kb=kb, w=w, Sct=Sct, iwt=iwt: e.scalar_tensor_tensor(out=Sct[:, kb * 512:kb * 512 + w], in0=rl[:, 0:w], scalar=iwt[:, 8 + h:9 + h],
                                                                                                                            in1=Sct[:, kb * 512:kb * 512 + w], op0=ALU.mult, op1=ALU.add),
                                                         reads=["rl", "iws" + sfx, kS], writes=[kS])
                                            yield
                                        P.op("dve", lambda e, nk=nk, Sct=Sct, mn=mn: e.tensor_reduce(out=mn, in_=Sct[:, 0:nk], axis=AX.X, op=ALU.min), reads=[kS], writes=["mn" + sfx])
                                        P.op("dve", lambda e, qi=qi, Sct=Sct: e.tensor_tensor(out=Sct[:, qi * 128:(qi + 1) * 128], in0=Sct[:, qi * 128:(qi + 1) * 128], in1=caus_b, op=ALU.add),
                                             reads=[kS, "cf"], writes=[kS])
                                        P.op("dve", lambda e, nk=nk, Sct=Sct, mx=mx: e.tensor_reduce(out=mx, in_=Sct[:, 0:nk], axis=AX.X, op=ALU.max), reads=[kS], writes=["mx" + sfx])
                                        P.op("dve", lambda e, mx=mx, mn=mn, w0=w0: e.tensor_tensor(out=w0, in0=mx, in1=mn, op=ALU.subtract), reads=["mx" + sfx, "mn" + sfx], writes=["w0" + sfx])
                                        P.op("dve", lambda e, bt=bt, w0=w0: e.tensor_scalar(out=bt[:, 0:NBIS + 1], in0=pow2, scalar1=w0, scalar2=None, op0=ALU.mult), reads=["w0" + sfx, "cf"], writes=["hk" + sfx])
                                        P.op("dve", lambda e, bt=bt, mn=mn, mid=mid: e.tensor_tensor(out=mid, in0=mn, in1=bt[:, 0:1], op=ALU.add), reads=["mn" + sfx, "hk" + sfx], writes=["mid" + sfx])
                                        yield
                                    if tt % 2 == 0:
                                        continue
                                    pair = [t for t in (tt - 1, tt) if 4 * j + t >= 2]
                                    for k in range(NBIS):
                                        for t in pair:
                                            f_ = str(t % 2)
                                            bt2, nk2 = bis[t % 2], 128 * (4 * j + t + 1)
                                            if t % 2 == 0 or not ACT_COUNT:
                                                P.op("dve", lambda e, bt2=bt2, nk2=nk2, t=t: e.tensor_scalar(out=junk[:, 0:nk2], in0=Sc[t % 2][:, 0:nk2], scalar1=bt2[:, NBIS + 5:NBIS + 6], scalar2=None,
                                                                                                         op0=ALU.is_ge, op1=ALU.add, accum_out=bt2[:, NBIS + 7:NBIS + 8]),
                                                     reads=["Sc" + f_, "mid" + f_], writes=["cnt" + f_])
                                            else:
                                                P.op("act", lambda e, bt2=bt2, nk2=nk2, t=t: e.activation(out=rl[:, :].bitcast(U8)[:, 0:nk2], in_=Sc[t % 2][:, 0:nk2], func=AF.Sign, scale=-1.0,
                                                                                                      bias=bt2[:, NBIS + 5:NBIS + 6], accum_out=bt2[:, NBIS + 7:NBIS + 8]),
                                                     reads=["Sc" + f_, "mid" + f_], writes=["cnt" + f_, "rl"])
                                        for t in pair:
                                            f_ = str(t % 2)
                                            bt2, nk2 = bis[t % 2], 128 * (4 * j + t + 1)
                                            if t % 2 == 0 or not ACT_COUNT:
                                                P.op("dve", lambda e, bt2=bt2, k=k: e.tensor_scalar(out=bt2[:, NBIS + 6:NBIS + 7], in0=bt2[:, NBIS + 7:NBIS + 8], scalar1=256.0, scalar2=bt2[:, k:k + 1], op0=ALU.is_ge, op1=ALU.mult),
                                                     reads=["cnt" + f_, "hk" + f_], writes=["tq" + f_])
                                            else:
                                                P.op("dve", lambda e, bt2=bt2, k=k, nk2=nk2: e.tensor_scalar(out=bt2[:, NBIS + 6:NBIS + 7], in0=bt2[:, NBIS + 7:NBIS + 8], scalar1=float(nk2 - 512), scalar2=bt2[:, k:k + 1], op0=ALU.is_le, op1=ALU.mult),
                                                     reads=["cnt" + f_, "hk" + f_], writes=["tq" + f_])
                                        for t in pair:
                                            f_ = str(t % 2)
                                            bt2 = bis[t % 2]
                                            P.op("dve", lambda e, bt2=bt2, k=k: e.scalar_tensor_tensor(out=bt2[:, NBIS + 5:NBIS + 6], in0=bt2[:, NBIS + 6:NBIS + 7], scalar=bt2[:, k + 1:k + 2], in1=bt2[:, NBIS + 5:NBIS + 6],
                                                                                                     op0=ALU.subtract, op1=ALU.add),
                                                 reads=["tq" + f_, "hk" + f_, "mid" + f_], writes=["mid" + f_])
                                        if pair:
                                            yield
                                    for t in pair:
                                        f_ = str(t % 2)
                                        bt2 = bis[t % 2]
                                        P.op("dve", lambda e, bt2=bt2: e.tensor_tensor(out=bt2[:, NBIS + 4:NBIS + 5], in0=bt2[:, NBIS + 5:NBIS + 6], in1=bt2[:, NBIS:NBIS + 1], op=ALU.subtract),
                                             reads=["mid" + f_, "hk" + f_], writes=["thr" + f_])

                                    def emit_masks(tiles, j=j, par=par):
                                        for t in tiles:
                                            f_ = str(t % 2)
                                            bt2, nk2 = bis[t % 2], 128 * (4 * j + t + 1)
                                            for kb in range((nk2 + 511) // 512):
                                                w = min(512, nk2 - kb * 512)
                                                mj = kb % 2
                                                P.op("dve", lambda e, kb=kb, w=w, mj=mj, t=t, bt2=bt2: e.tensor_scalar(out=mk[:, mj, 0:w], in0=Sc[t % 2][:, kb * 512:kb * 512 + w], scalar1=bt2[:, NBIS + 4:NBIS + 5], scalar2=None, op0=ALU.is_ge),
                                                     reads=["Sc" + f_, "thr" + f_], writes=["mk%d" % mj])
                                                nt_ = w // 128
                                                for q in range(nt_):
                                                    P.op("pe", lambda e, q=q, mj=mj: e.transpose(out=PT[1][:, q * 128:(q + 1) * 128], in_=mk[:, mj, q * 128:(q + 1) * 128], identity=ident_b[:]),
                                                         reads=["mk%d" % mj, "ident_b"], writes=[ptk(1)])
                                                P.op("act", lambda e, kb=kb, nt_=nt_, t=t, par=par: e.activation(out=maskT[:, par, kb * 4:kb * 4 + nt_, t * 128:(t + 1) * 128],
                                                                                                       in_=PT[1][:, 0:nt_ * 128].rearrange("p (c t) -> p c t", t=128), func=AF.Copy),
                                                     reads=[ptk(1)], writes=["maskT%d" % par])
                                                yield

                                    if tt == 1:
                                        pend_masks.append(emit_masks((0, 1)))
                                    else:
                                        yield
                                        yield
                                        yield from emit_masks((2, 3))

                            def att_gen(j):
                                par = j % 2
                                mT_ = maskT[:, par] if mixer == "A" else None
                                yield from attention(mixer, l, j, kT, v, qT[:, par], mT_, yT, PTt, rD, ysb, sqb, ratio=(8 if mixer == "A" else 16))

                            for _ in stage1(0):
                                pass
                            for j in range(4):
                                ga = att_gen(j)
                                gs = stage1(j + 1) if j < 3 else iter(())
                                n_att = (8 * (4 * j + 4)) // (8 if mixer == "A" else 16)
                                n_s1 = 100 if mixer == "A" else 5
                                kadv = max(1, -(-n_s1 // max(1, n_att - 1)))
                                for _ in ga:
                                    for _k in range(kadv):
                                        next(gs, None)
                                for _ in gs:
                                    pass
                            if dbg and b == 0 and l == 0:
                                dump("dbg_y" + mixer, yT[:], [128, 4, S], BF16, ["yT"])
                            P.barrier()
                            P.emit()

                    with contextlib.ExitStack() as ph2:
                        psb2 = lambda n, s, d: ph2.enter_context(nc.sbuf_tensor(uname(n), s, d))
                        hT = psb2("hTm", [128, 8, 1024], BF16)
                        mT = psb2("mT", [128, 8, 1024], BF16)
                        sq = psb2("sqm", [128, 2, 512], BF16)
                        rs = psb2("rsm", [128, 512], F32)
                        Wg = psb2("Wg", [128, 2, 8, 256], BF16)
                        Wu = psb2("Wu", [128, 2, 2, 4, 128], BF16)
                        Wo = psb2("Wo", [128, 2, 8, 128], BF16)
                        sa = psb2("sa", [128, 2, 512], F32)
                        m1 = psb2("m1", [128, 2, 512], F32)
                        for half in range(2):
                            for bb in range(2):
                                norm_block(gattn, l, half * 1024 + bb * 512, 512, hT[:, :, bb * 512:(bb + 1) * 512], "hTm", sq, rs, "m")
                            for dc in range(8):
                                wj = dc % 2
                                for gi in range(2):
                                    wload(Wg[:, wj, :, gi * 128:(gi + 1) * 128], win_view(l, O_G + gi * 1024 + dc * 128, 128), "Wg%d" % wj, "Wg%d" % wj)
                                wload(Wu[:, wj, 0, :, :], wua_d[l].rearrange("(kc p) n -> p kc n", p=128)[:, :, dc * 128:(dc + 1) * 128], "Wu%d" % wj, "Wu%d" % wj)
                                wload(Wu[:, wj, 1, :, :], wub_d[l].rearrange("(kc p) n -> p kc n", p=128)[:, :, dc * 128:(dc + 1) * 128], "Wu%d" % wj, "Wu%d" % wj)
                                for bb in range(2):
                                    t0 = half * 1024 + bb * 512
                                    for gi in range(2):
                                        for kc in range(8):
                                            P.op("pe", lambda e, gi=gi, kc=kc, wj=wj, bb=bb: e.matmul(PS[gi][:, :], lhsT=Wg[:, wj, kc, gi * 128:(gi + 1) * 128], rhs=hT[:, kc, bb * 512:(bb + 1) * 512],
                                                                                                   start=(kc == 0), stop=(kc == 7)),
                                                 reads=["hTm", "Wg%d" % wj], writes=[psk(gi)])
                                        yy = yaT if gi == 0 else ybT
                                        for kc in range(4):
                                            P.op("pe", lambda e, gi=gi, kc=kc, wj=wj, yy=yy, t0=t0: e.matmul(PS[2 + gi][:, :], lhsT=Wu[:, wj, gi, kc, :], rhs=yy[:, kc, t0:t0 + 512],
                                                                                                         start=(kc == 0), stop=(kc == 3)),
                                                 reads=["yaT" if gi == 0 else "ybT", "Wu%d" % wj], writes=[psk(2 + gi)])
                                        P.op("act", lambda e, gi=gi, dc=dc: e.activation(out=sa[:, gi, :], in_=PS[gi][:, :], func=AF.Sigmoid, bias=gbias[:, l, gi * 8 + dc:gi * 8 + dc + 1]),
                                             reads=[psk(gi), "gbias"], writes=["sa%d" % gi])
                                        P.op("dve", lambda e, gi=gi: e.tensor_tensor(out=m1[:, gi, :], in0=sa[:, gi, :], in1=PS[2 + gi][:, :], op=ALU.mult),
                                             reads=["sa%d" % gi, psk(2 + gi)], writes=["m1%d" % gi])
                                    P.op("dve", lambda e, dc=dc, bb=bb: e.tensor_tensor(out=mT[:, dc, bb * 512:(bb + 1) * 512], in0=m1[:, 0, :], in1=m1[:, 1, :], op=ALU.add),
                                         reads=["m10", "m11"], writes=["mT"])
                            for dc in range(8):
                                wj = dc % 2
                                wload(Wo[:, wj, :, :], wo_d[l].rearrange("(kc p) n -> p kc n", p=128)[:, :, dc * 128:(dc + 1) * 128], "Wo%d" % wj, "Wo%d" % wj)
                                for bb in range(2):
                                    t0 = half * 1024 + bb * 512
                                    bank = 4 + bb
                                    for kc in range(8):
                                        P.op("pe", lambda e, kc=kc, wj=wj, bb=bb, bank=bank: e.matmul(PS[bank][:, :], lhsT=Wo[:, wj, kc, :], rhs=mT[:, kc, bb * 512:(bb + 1) * 512],
                                                                                                  start=(kc == 0), stop=(kc == 7)),
                                             reads=["mT", "Wo%d" % wj], writes=[psk(bank)])
                                    P.op("dve", lambda e, dc=dc, t0=t0, bank=bank: e.tensor_tensor(out=xT[:, dc, t0:t0 + 512], in0=xT[:, dc, t0:t0 + 512], in1=PS[bank][:, :], op=ALU.add),
                                         reads=["xT", psk(bank)], writes=["xT"])
                            P.barrier()
                        if b == 0 and l == 0:
                            dump("dbg_x1", xT[:], [128, 8, S], F32, ["xT"])
                            P.barrier()
                        P.emit()

                with contextlib.ExitStack() as ph:
                    psb = lambda n, s, d: ph.enter_context(nc.sbuf_tensor(uname(n), s, d))
                    h2 = psb("h2", [128, 8, S], BF16)
                    sq = psb("sqf", [128, 2, 512], BF16)
                    rs = psb("rsf", [128, 512], F32)
                    Wfi = psb("Wfi", [128, 2, 8, 2, 512], BF16)
                    Wfo = psb("Wfo", [128, 2, 4, D], BF16)
                    sg = psb("sg", [128, 2, 512], F32)
                    act = psb("actf", [128, 4, 512], BF16)
                    for blk in range(4):
                        norm_block(gffn, l, blk * 512, 512, h2[:, :, blk * 512:(blk + 1) * 512], "h2", sq, rs, "f")
                    groups = [(g0, min(4, 22 - g0)) for g0 in range(0, 22, 4)]
                    for gi, (g0, G) in enumerate(groups):
                        wj = gi % 2
                        for gu in range(2):
                            wload(Wfi[:, wj, :, gu, 0:G * 128], wfi_d[l].rearrange("(kc p) n -> p kc n", p=128)[:, :, gu * FH + g0 * 128:gu * FH + (g0 + G) * 128], "Wfi%d" % wj, "Wfi%d" % wj)
                        wload(Wfo[:, wj, 0:G, :], wfo_d[l][g0 * 128:(g0 + G) * 128, :].rearrange("(g p) n -> p g n", p=128), "Wfo%d" % wj, "Wfo%d" % wj)
                        for blk in range(4):
                            for g in range(G):
                                for gu in range(2):
                                    for kc in range(8):
                                        P.op("pe", lambda e, g=g, gu=gu, kc=kc, wj=wj, blk=blk: e.matmul(PS[gu][:, :], lhsT=Wfi[:, wj, kc, gu, g * 128:(g + 1) * 128], rhs=h2[:, kc, blk * 512:(blk + 1) * 512],
                                                                                                     start=(kc == 0), stop=(kc == 7)),
                                             reads=["h2", "Wfi%d" % wj], writes=[psk(gu)])
                                sj = g % 2
                                P.op("act", lambda e, sj=sj: e.activation(out=sg[:, sj, :], in_=PS[0][:, :], func=AF.Silu), reads=[psk(0)], writes=["sg%d" % sj])
                                P.op("dve", lambda e, sj=sj, g=g: e.tensor_tensor(out=act[:, g, :], in0=sg[:, sj, :], in1=PS[1][:, :], op=ALU.mult),
                                     reads=["sg%d" % sj, psk(1)], writes=["act%d" % g])
                            for dc in range(8):
                                bank = 2 + dc % 4
                                for g in range(G):
                                    P.op("pe", lambda e, g=g, dc=dc, wj=wj, bank=bank: e.matmul(PS[bank][:, :], lhsT=Wfo[:, wj, g, dc * 128:(dc + 1) * 128], rhs=act[:, g, :],
                                                                                            start=(g == 0), stop=(g == G - 1)),
                                         reads=["act%d" % g, "Wfo%d" % wj], writes=[psk(bank)])
                                P.op("dve", lambda e, dc=dc, blk=blk, bank=bank: e.tensor_tensor(out=xT[:, dc, blk * 512:(blk + 1) * 512], in0=xT[:, dc, blk * 512:(blk + 1) * 512], in1=PS[bank][:, :], op=ALU.add),
                                     reads=["xT", psk(bank)], writes=["xT"])
                    P.barrier()
                    P.emit()

            with contextlib.ExitStack() as ph:
                psb = lambda n, s, d: ph.enter_context(nc.sbuf_tensor(uname(n), s, d))
                xo = psb("xo", [128, 2, D], F32)
                for t in range(NT):
                    jx = t % 2
                    for half in range(2):
                        bank = (2 * t + half) % 4
                        for q in range(4):
                            c = half * 4 + q
                            P.op("pe", lambda e, t=t, c=c, q=q, bank=bank: e.transpose(out=PS[bank][:, q * 128:(q + 1) * 128], in_=xT[:, c, t * 128:(t + 1) * 128], identity=ident_f),
                                 reads=["xT", "cf"], writes=[psk(bank)])
                        if half == 0:
                            P.op("act", lambda e, jx=jx, bank=bank: e.activation(out=xo[:, jx, 0:512], in_=PS[bank][:, :], func=AF.Copy), reads=[psk(bank)], writes=["xo%d" % jx])
                        else:
                            P.op("dve", lambda e, jx=jx, bank=bank: e.tensor_copy(out=xo[:, jx, 512:1024], in_=PS[bank][:, :]), reads=[psk(bank)], writes=["xo%d" % jx])
                    o = P.op("sp", lambda e, t=t, jx=jx: e.dma_start(out=out_d[b, t * 128:(t + 1) * 128, :], in_=xo[:, jx, :]), reads=["xo%d" % jx], dma="xo%d" % jx)
                    P.final.append(o)
                P.barrier()
                P.emit(final=(b == n_seq - 1))
    return nc


def host_consts(depth, inputs):
    cfa = np.zeros((128, 128 * 3 + 32 + NBIS + 1), np.float32)
    cfa[:, 0:128] = np.eye(128, dtype=np.float32)
    qq, kk = np.meshgrid(np.arange(128), np.arange(128), indexing="ij")
    cfa[:, 128:256] = np.where(kk <= qq, 0.0, -1e30)
    cfa[:, 256:384] = (qq <= kk).astype(np.float32)
    cfa[:, 384:416] = (1.0 / (10000.0 ** (np.arange(0, 64, 2, dtype=np.float32) / 64.0))).astype(np.float32)[None, :]
    cfa[:, 416:416 + NBIS + 1] = (0.5 ** np.arange(1, NBIS + 2)).astype(np.float32)[None, :]
    fm = lambda a, nch: np.ascontiguousarray(np.asarray(a, np.float32).reshape(depth, nch, 128).transpose(2, 0, 1))
    rep = lambda a: np.ascontiguousarray(np.broadcast_to(np.asarray(a, np.float32)[None], (128,) + np.asarray(a).shape))
    hnorm = np.stack([inputs[k] for k in ("a_q_norm", "a_k_norm", "idx_k_norm", "b_q_norm", "b_k_norm")], axis=1)
    return {
        "cf32": cfa,
        "gattn": fm(inputs["attn_norm"], 8),
        "gffn": fm(inputs["ffn_norm"], 8),
        "gbias": fm(inputs["gate_bias"], 16),
        "hnorm": rep(hnorm),
        "subln": np.ascontiguousarray(np.asarray(inputs["b_subln"], np.float32).T),
        "dlam": rep(inputs["diff_lambda"]),
    }


def run(inputs, n_cores=8, n_seq=4, depth=2, dbg=False):
    inputs = {k: np.asarray(v) for k, v in inputs.items()}
    inputs = {k: (v if k in ("x", "positions") else v[:depth]) for k, v in inputs.items()}
    nc = build(n_seq, depth, dbg)
    common = host_consts(depth, inputs)
    for k in ("w_in", "w_up_a", "w_up_b", "w_out", "w_ffn_in", "w_ffn_out"):
        common[k] = np.ascontiguousarray(inputs[k][:depth], dtype=np.float32)
    in_maps = []
    for c in range(n_cores):
        m = dict(common)
        m["x"] = np.ascontiguousarray(inputs["x"][c * n_seq:(c + 1) * n_seq], dtype=np.float32)
        p = np.asarray(inputs["positions"][c * n_seq:(c + 1) * n_seq], np.int32)
        m["pos"] = np.ascontiguousarray(p.reshape(n_seq, NT, 128).transpose(0, 2, 1))
        in_maps.append(m)
    res = run_bass_kernel_spmd(nc, in_maps, core_ids=list(range(n_cores)))
    if dbg:
        return res.results
    return np.concatenate([r["out"] for r in res.results], axis=0)


def kernel(**inputs):
    return run(inputs, 8, 4, 2).astype(np.float32)
```

```python
import contextlib
import math
import numpy as np
import concourse.bass as bass
import concourse.mybir as mybir
from concourse.bass_utils import run_bass_kernel_spmd

dt = mybir.dt
F32, BF16, I32, U8 = dt.float32, dt.bfloat16, dt.int32, dt.uint8
ALU, AF, AX = mybir.AluOpType, mybir.ActivationFunctionType, mybir.AxisListType

S = 2048
D = 1024
DIN = 5444
FH = 2816
NT = 16
EPS = 1e-6
NBIS = 16
ACT_COUNT = True
EPOCH = 6000
ENGS = ("pe", "dve", "act", "pool", "sp")
ENGMAP = {"pe": "tensor", "dve": "vector", "act": "scalar", "pool": "gpsimd", "sp": "sync"}
O_AQ, O_AK, O_AV, O_IQ, O_IK, O_IW, O_BQ, O_BK, O_BV, O_G = 0, 512, 1024, 1536, 1792, 1856, 1860, 2372, 2884, 3396


class Op:
    __slots__ = ("eng", "fn", "deps", "sem", "val", "is_dma")

    def __init__(self, eng, fn, is_dma=False):
        self.eng, self.fn, self.is_dma = eng, fn, is_dma
        self.deps, self.sem, self.val = [], None, None


class Prog:
    def __init__(self, nc, stack):
        self.nc, self.stack = nc, stack
        self.pending = {e: [] for e in ENGS}
        self.cnt = {e: 0 for e in ENGS}
        self.last_op = {e: None for e in ENGS}
        self.dmas = []
        self.slot_cnt = {}
        self.sems = {}
        self.waited = {e: {} for e in ENGS}
        self.last_w, self.readers = {}, {}
        self.final = []

    def _sem(self, name):
        if name not in self.sems:
            self.sems[name] = self.stack.enter_context(self.nc.semaphore(name))
        return self.sems[name]

    def op(self, eng, fn, reads=(), writes=(), dma=None):
        o = Op(eng, fn, is_dma=dma is not None)
        deps = set()
        for r in reads:
            w = self.last_w.get(r)
            if w is not None:
                deps.add(w)
        for k in writes:
            w = self.last_w.get(k)
            if w is not None and (w.eng != eng or w.is_dma or o.is_dma):
                deps.add(w)
            for rd in self.readers.get(k, ()):
                if rd.eng != eng or rd.is_dma or o.is_dma:
                    deps.add(rd)
        o.deps = list(deps)
        for r in reads:
            self.readers.setdefault(r, []).append(o)
        for k in writes:
            self.last_w[k] = o
            self.readers[k] = []
        if o.is_dma:
            c = self.slot_cnt.get(dma, 0)
            ep, c2 = c // EPOCH, c % EPOCH + 16
            if c2 > EPOCH:
                ep, c2 = ep + 1, 16
                c = ep * EPOCH
            self.slot_cnt[dma] = ep * EPOCH + c2
            o.sem, o.val = self._sem("d%s_%d" % (dma, ep)), c2
            self.dmas.append(o)
        else:
            c = self.cnt[eng]
            o.sem, o.val = self._sem("e%s_%d" % (eng, c // EPOCH)), c % EPOCH + 1
            self.cnt[eng] = c + 1
            self.last_op[eng] = o
        self.pending[eng].append(o)
        return o

    def barrier(self):
        lasts = [self.last_op[e] for e in ENGS if self.last_op[e] is not None] + list(self.dmas)
        for e in ENGS:
            o = Op(e, None)
            o.deps = lasts
            self.pending[e].append(o)
        self.dmas = []
        self.last_w, self.readers = {}, {}

    def emit(self, final=False):
        with self.nc.Block() as block:
            for e in ENGS:
                ops = self.pending[e]
                if not ops and not (final and e == "sp"):
                    continue

                def body(eng, ops=ops, e=e):
                    waited = self.waited[e]
                    for o in ops:
                        need = {}
                        for d in o.deps:
                            k = id(d.sem)
                            if waited.get(k, 0) >= d.val:
                                continue
                            if k not in need or need[k][1] < d.val:
                                need[k] = (d.sem, d.val)
                        for k, (s, v) in need.items():
                            eng.wait_ge(s, v)
                            waited[k] = v
                        if o.fn is None:
                            continue
                        ins = o.fn(eng)
                        ins.then_inc(o.sem, 16 if o.is_dma else 1)
                    if final and e == "sp":
                        for o in self.final:
                            eng.wait_ge(o.sem, o.val)

                getattr(block, ENGMAP[e])(body)
        self.pending = {e: [] for e in ENGS}


_uid = [0]


def uname(n):
    _uid[0] += 1
    return "%s_%d" % (n, _uid[0])


def build(n_seq, depth, dbg=False):
    nc = bass.Bass("TRN2", target_bir_lowering=False, dynamic_dma_scratch_size=4096)
    dram = lambda n, s, d, k="ExternalInput": nc.dram_tensor(n, s, d, kind=k).ap()
    x_d = dram("x", [n_seq, S, D], F32)
    pos_d = dram("pos", [n_seq, 128, NT], I32)
    win_d = dram("w_in", [depth, D, DIN], F32)
    wua_d = dram("w_up_a", [depth, 512, D], F32)
    wub_d = dram("w_up_b", [depth, 512, D], F32)
    wo_d = dram("w_out", [depth, D, D], F32)
    wfi_d = dram("w_ffn_in", [depth, D, 2 * FH], F32)
    wfo_d = dram("w_ffn_out", [depth, FH, D], F32)
    gattn_d = dram("gattn", [128, depth, 8], F32)
    gffn_d = dram("gffn", [128, depth, 8], F32)
    gbias_d = dram("gbias", [128, depth, 16], F32)
    hn_d = dram("hnorm", [128, depth, 5, 64], F32)
    subln_d = dram("subln", [128, depth], F32)
    dlam_d = dram("dlam", [128, depth, 4, 64], F32)
    cf_d = dram("cf32", [128, 128 * 3 + 32 + NBIS + 1], F32)
    out_d = dram("out", [n_seq, S, D], F32, "ExternalOutput")

    with contextlib.ExitStack() as st:
        sb = lambda n, s, d: st.enter_context(nc.sbuf_tensor(uname(n), s, d))
        P = Prog(nc, st)

        def dump(name, src, shape, dty, keys):
            if not dbg:
                return
            dd = nc.dram_tensor(name, shape, dty, kind="ExternalOutput").ap()
            o = P.op("sp", lambda e: e.dma_start(out=dd, in_=src), reads=keys, dma="dbg")
            P.final.append(o)
        xT = sb("xT", [128, 8, S], F32)
        cf = sb("cf", [128, 128 * 3 + 32 + NBIS + 1], F32)
        ident_f, caus_b, invf = cf[:, 0:128], cf[:, 128:256], cf[:, 384:416]
        pow2 = cf[:, 416:416 + NBIS + 1]
        ident_b = sb("ident_b", [128, 128], BF16)
        triT_b = sb("triT_b", [128, 128], BF16)
        ones_b = sb("ones_b", [128, 128], BF16)
        negI_b = sb("negI_b", [128, 128], BF16)
        gattn = sb("gattn_s", [128, depth, 8], F32)
        gffn = sb("gffn_s", [128, depth, 8], F32)
        gbias = sb("gbias_s", [128, depth, 16], F32)
        hn = sb("hn_s", [128, depth, 5, 64], F32)
        subln = sb("subln_s", [128, depth], F32)
        lam = sb("lam_s", [128, depth], F32)
        nlam = sb("nlam_s", [128, depth], F32)
        cosT = sb("cosT", [128, NT, 32], F32)
        sinT = sb("sinT", [128, NT, 32], F32)
        PS = [st.enter_context(nc.psum_tensor("ps%d" % i, [128, 512], F32)) for i in range(6)]
        PT = [st.enter_context(nc.psum_tensor("pt%d" % i, [128, 1024], BF16)) for i in range(2)]
        psk = lambda i: "ps%d" % i
        ptk = lambda i: "pt%d" % i

        cst = contextlib.ExitStack()
        dlam = cst.enter_context(nc.sbuf_tensor(uname("dlam_s"), [128, depth, 4, 64], F32))
        small = cst.enter_context(nc.sbuf_tensor(uname("small"), [128, 80], F32))
        P.op("sp", lambda e: e.dma_start(out=cf[:], in_=cf_d[:, :]), writes=["cf"], dma="c0")
        for nm, t, d_ in (("gattn", gattn, gattn_d), ("gffn", gffn, gffn_d), ("gbias", gbias, gbias_d),
                          ("hn", hn, hn_d), ("subln", subln, subln_d), ("dlam", dlam, dlam_d)):
            P.op("sp", lambda e, t=t, d_=d_: e.dma_start(out=t[:], in_=d_), writes=[nm], dma="c_" + nm)
        P.op("dve", lambda e: e.tensor_copy(out=ident_b[:], in_=cf[:, 0:128]), reads=["cf"], writes=["ident_b"])
        P.op("dve", lambda e: e.tensor_copy(out=triT_b[:], in_=cf[:, 256:384]), reads=["cf"], writes=["triT_b"])
        P.op("dve", lambda e: e.memset(ones_b[:], 1.0), writes=["ones_b"])
        P.op("dve", lambda e: e.tensor_scalar(out=negI_b[:], in0=cf[:, 0:128], scalar1=-30000.0, scalar2=None, op0=ALU.mult), reads=["cf"], writes=["negI_b"])
        for l in range(depth):
            lam_init = 0.8 - 0.6 * math.exp(-0.3 * l)
            P.op("dve", lambda e, l=l: e.tensor_tensor(out=small[:, 0:64], in0=dlam[:, l, 0, :], in1=dlam[:, l, 1, :], op=ALU.mult),
                 reads=["dlam"], writes=["sm0"])
            P.op("dve", lambda e: e.tensor_reduce(out=small[:, 64:65], in_=small[:, 0:64], axis=AX.X, op=ALU.add),
                 reads=["sm0"], writes=["sm0"])
            P.op("act", lambda e: e.activation(out=small[:, 65:66], in_=small[:, 64:65], func=AF.Exp), reads=["sm0"], writes=["sm1"])
            P.op("dve", lambda e, l=l: e.tensor_tensor(out=small[:, 0:64], in0=dlam[:, l, 2, :], in1=dlam[:, l, 3, :], op=ALU.mult),
                 reads=["dlam", "sm1"], writes=["sm0"])
            P.op("dve", lambda e: e.tensor_reduce(out=small[:, 64:65], in_=small[:, 0:64], axis=AX.X, op=ALU.add),
                 reads=["sm0"], writes=["sm0"])
            P.op("act", lambda e: e.activation(out=small[:, 66:67], in_=small[:, 64:65], func=AF.Exp), reads=["sm0"], writes=["sm2"])
            P.op("dve", lambda e, l=l, li=lam_init: e.scalar_tensor_tensor(out=lam[:, l:l + 1], in0=small[:, 65:66], scalar=li,
                                                                          in1=small[:, 66:67], op0=ALU.add, op1=ALU.subtract),
                 reads=["sm1", "sm2"], writes=["lam"])
            P.op("dve", lambda e, l=l: e.tensor_scalar(out=nlam[:, l:l + 1], in0=lam[:, l:l + 1], scalar1=-1.0, scalar2=None, op0=ALU.mult),
                 reads=["lam"], writes=["nlam"])
        P.barrier()
        P.emit()
        cst.close()

        def wload(dst, src, key, slot):
            return P.op("pool", lambda e: e.dma_start(out=dst, in_=src), writes=[key], dma=slot)

        def win_view(l, c0, n):
            return win_d[l].rearrange("(kc p) n -> p kc n", p=128)[:, :, c0:c0 + n]

        def norm_block(g, l, t0, n, dst, dkey, sq, rs, tag, nb=5):
            for c in range(8):
                j = c % 2
                P.op("act", lambda e, c=c, j=j: e.activation(out=sq[:, j, 0:n], in_=xT[:, c, t0:t0 + n], func=AF.Square),
                     reads=["xT"], writes=[tag + "sq%d" % j])
                P.op("pe", lambda e, c=c, j=j: e.matmul(PS[nb][:, 0:n], lhsT=ones_b[:], rhs=sq[:, j, 0:n], start=(c == 0), stop=(c == 7)),
                     reads=[tag + "sq%d" % j, "ones_b"], writes=[psk(nb)])
            P.op("act", lambda e: e.activation(out=rs[:, 0:n], in_=PS[nb][:, 0:n], func=AF.Ln, scale=1.0 / D, bias=eps_t[:, 0:1]),
                 reads=[psk(nb), "eps"], writes=[tag + "rs"])
            P.op("act", lambda e: e.activation(out=rs[:, 0:n], in_=rs[:, 0:n], func=AF.Exp, scale=-0.5), reads=[tag + "rs"], writes=[tag + "rs"])
            for c in range(8):
                P.op("dve", lambda e, c=c: e.scalar_tensor_tensor(out=dst[:, c, 0:n], in0=xT[:, c, t0:t0 + n], scalar=g[:, l, c:c + 1],
                                                                   in1=rs[:, 0:n], op0=ALU.mult, op1=ALU.mult),
                     reads=["xT", tag + "rs", "gains"], writes=[dkey])

        eps_t = sb("eps_t", [128, 1], F32)
        P.op("dve", lambda e: e.memset(eps_t[:], EPS), writes=["eps"])

        def proj_tok(hT, hkey, tt, W, wkey, ncols, bank):
            for kc in range(8):
                P.op("pe", lambda e, kc=kc: e.matmul(PS[bank][:, 0:ncols], lhsT=hT[:, kc, tt * 128:(tt + 1) * 128], rhs=W[:, kc, 0:ncols],
                                                     start=(kc == 0), stop=(kc == 7)),
                     reads=[hkey, wkey], writes=[psk(bank)])

        def postproc(src, skey, H, gain, tile, out, okey, scr, do_norm=True, sfx=""):
            t_sq, t_n, t_a, t_b, ss = scr
            W = H * 64
            ksq, kn, ka, kb2, kss = "t_sq" + sfx, "t_n" + sfx, "t_a" + sfx, "t_b" + sfx, "ss" + sfx
            v3 = lambda ap: ap.rearrange("p (h d) -> p h d", d=64)
            if do_norm:
                P.op("act", lambda e: e.activation(out=t_sq[:, 0:W], in_=src, func=AF.Square), reads=[skey], writes=[ksq])
                P.op("dve", lambda e: e.tensor_reduce(out=ss[:, 0:H], in_=v3(t_sq[:, 0:W]), axis=AX.X, op=ALU.add),
                     reads=[ksq], writes=[kss])
                P.op("act", lambda e: e.activation(out=ss[:, 0:H], in_=ss[:, 0:H], func=AF.Ln, scale=1.0 / 64, bias=eps_t[:, 0:1]), reads=[kss, "eps"], writes=[kss])
                P.op("act", lambda e: e.activation(out=ss[:, 0:H], in_=ss[:, 0:H], func=AF.Exp, scale=-0.5), reads=[kss], writes=[kss])
                P.op("dve", lambda e: e.tensor_tensor(out=v3(t_n[:, 0:W]), in0=v3(src), in1=ss[:, 0:H].unsqueeze(2).to_broadcast([128, H, 64]), op=ALU.mult),
                     reads=[skey, kss], writes=[kn])
                P.op("dve", lambda e: e.tensor_tensor(out=v3(t_n[:, 0:W]), in0=v3(t_n[:, 0:W]), in1=gain.unsqueeze(1).to_broadcast([128, H, 64]), op=ALU.mult),
                     reads=[kn, "gains"], writes=[kn])
            else:
                P.op("act", lambda e: e.activation(out=t_n[:, 0:W], in_=src, func=AF.Copy), reads=[skey], writes=[kn])
            x1, x2 = v3(t_n[:, 0:W])[:, :, 0:32], v3(t_n[:, 0:W])[:, :, 32:64]
            o1, o2 = v3(out)[:, :, 0:32], v3(out)[:, :, 32:64]
            a3 = t_a[:, 0:H * 32].rearrange("p (h d) -> p h d", d=32)
            b3 = t_b[:, 0:H * 32].rearrange("p (h d) -> p h d", d=32)
            a4 = t_a[:, 256:256 + H * 32].rearrange("p (h d) -> p h d", d=32)
            b4 = t_b[:, 256:256 + H * 32].rearrange("p (h d) -> p h d", d=32)
            cs = cosT[:, tile, :].unsqueeze(1).to_broadcast([128, H, 32])
            sn = sinT[:, tile, :].unsqueeze(1).to_broadcast([128, H, 32])
            P.op("pool", lambda e: e.tensor_tensor(out=a3, in0=x1, in1=cs, op=ALU.mult), reads=[kn, "tab"], writes=[ka])
            P.op("pool", lambda e: e.tensor_tensor(out=b3, in0=x2, in1=sn, op=ALU.mult), reads=[kn, "tab"], writes=[kb2])
            P.op("pool", lambda e: e.tensor_tensor(out=a4, in0=x2, in1=cs, op=ALU.mult), reads=[kn, "tab"], writes=[ka + "2"])
            P.op("pool", lambda e: e.tensor_tensor(out=b4, in0=x1, in1=sn, op=ALU.mult), reads=[kn, "tab"], writes=[kb2 + "2"])
            P.op("pool", lambda e: e.tensor_tensor(out=o1, in0=a3, in1=b3, op=ALU.subtract), reads=[ka, kb2], writes=[okey])
            P.op("pool", lambda e: e.tensor_tensor(out=o2, in0=a4, in1=b4, op=ALU.add), reads=[ka + "2", kb2 + "2"], writes=[okey])

        def range_sin(src, shift, dst, ki, kf, r, m):
            twopi = 2.0 * math.pi
            C1, C2 = 6.28125, twopi - 6.28125
            k = "rs_"
            P.op("dve", lambda e: e.tensor_scalar(out=ki, in0=src, scalar1=1.0 / twopi, scalar2=shift / twopi, op0=ALU.mult, op1=ALU.add),
                 reads=["ang"], writes=[k + "ki"])
            P.op("dve", lambda e: e.tensor_copy(out=kf, in_=ki), reads=[k + "ki"], writes=[k + "kf"])
            P.op("dve", lambda e: e.scalar_tensor_tensor(out=r, in0=kf, scalar=-C1, in1=src, op0=ALU.mult, op1=ALU.add),
                 reads=[k + "kf", "ang"], writes=[k + "r"])
            P.op("dve", lambda e: e.scalar_tensor_tensor(out=r, in0=kf, scalar=-C2, in1=r, op0=ALU.mult, op1=ALU.add),
                 reads=[k + "kf", k + "r"], writes=[k + "r"])
            if shift:
                P.op("dve", lambda e: e.tensor_scalar(out=r, in0=r, scalar1=float(shift), scalar2=None, op0=ALU.add), reads=[k + "r"], writes=[k + "r"])
            P.op("dve", lambda e: e.tensor_scalar(out=m, in0=r, scalar1=math.pi, scalar2=twopi, op0=ALU.is_gt, op1=ALU.mult), reads=[k + "r"], writes=[k + "m"])
            P.op("dve", lambda e: e.tensor_tensor(out=r, in0=r, in1=m, op=ALU.subtract), reads=[k + "r", k + "m"], writes=[k + "r"])
            P.op("dve", lambda e: e.tensor_scalar(out=m, in0=r, scalar1=-math.pi, scalar2=twopi, op0=ALU.is_lt, op1=ALU.mult), reads=[k + "r"], writes=[k + "m"])
            P.op("dve", lambda e: e.tensor_tensor(out=r, in0=r, in1=m, op=ALU.add), reads=[k + "r", k + "m"], writes=[k + "r"])
            P.op("dve", lambda e: e.tensor_scalar(out=r, in0=r, scalar1=3.1415925, scalar2=-3.1415925, op0=ALU.min, op1=ALU.max), reads=[k + "r"], writes=[k + "r"])
            P.op("act", lambda e: e.activation(out=dst, in_=r, func=AF.Sin), reads=[k + "r"], writes=["tab"])

        def attention(mixer, l, j, kT, v, qT, maskT, yT, PTt, rD, ysb, sqb, ratio=4):
            nkt = 4 * j + 4
            steps = [(u, kt) for u in range(8) for kt in range(nkt)]
            qs = "qT%d" % (j % 2)

            def geo(i):
                u, kt = steps[i]
                d = kt - 4 * j
                qoff = max(d, 0) * 128
                return u, kt, d, qoff, i % 2, i % 3

            def emit_qk(i):
                u, kt, d, qoff, sbk, ptj = geo(i)
                c, r0 = u // 2, (u % 2) * 64
                if mixer == "A":
                    P.op("pe", lambda e: e.matmul(PS[sbk][:, qoff:512], lhsT=kT[r0:r0 + 64, c, kt * 128:(kt + 1) * 128], rhs=qT[r0:r0 + 64, c, qoff:512], start=True, stop=False),
                         reads=["kT", qs], writes=[psk(sbk)])
                    P.op("pe", lambda e: e.matmul(PS[sbk][:, qoff:512], lhsT=negI_b[:], rhs=maskT[:, kt, qoff:512], start=False, stop=True),
                         reads=["negI_b", "maskT%d" % (j % 2)], writes=[psk(sbk)])
                else:
                    P.op("pe", lambda e: e.matmul(PS[sbk][:, qoff:512], lhsT=kT[:, c, kt * 128:(kt + 1) * 128], rhs=qT[:, u % 2, c, qoff:512], start=True, stop=True),
                         reads=["kT", qs], writes=[psk(sbk)])

            emit_qk(0)
            for i in range(len(steps)):
                u, kt, d, qoff, sbk, ptj = geo(i)
                c, r0 = u // 2, (u % 2) * 64
                ob, db = (2, 3) if u % 2 == 0 else (4, 5)
                will_yield = ((i + 1) % ratio == 0)
                if i + 1 < len(steps) and not will_yield:
                    emit_qk(i + 1)
                pk = "PT%d" % ptj
                P.op("act", lambda e, sbk=sbk, ptj=ptj, qoff=qoff: e.activation(out=PTt[:, ptj, qoff:512], in_=PS[sbk][:, qoff:512], func=AF.Exp, scale=0.125),
                     reads=[psk(sbk)], writes=[pk])
                if mixer == "A":
                    pass
                elif d >= 0:
                    P.op("dve", lambda e, ptj=ptj, qoff=qoff: e.tensor_tensor(out=PTt[:, ptj, qoff:qoff + 128], in0=PTt[:, ptj, qoff:qoff + 128],
                                                                            in1=triT_b[:], op=ALU.mult),
                         reads=[pk, "triT_b"], writes=[pk])
                if mixer == "A":
                    lo = (u - 1) * 64 if u % 2 else u * 64
                    vst = v[:, kt, lo:lo + (128 if u % 2 else 64)]
                    M = 128 if u % 2 else 64
                else:
                    hh = u // 2
                    vst = v[:, kt, hh * 128:(hh + 1) * 128]
                    M = 128
                P.op("pe", lambda e, vst=vst, M=M, ptj=ptj, qoff=qoff, kt=kt, ob=ob: e.matmul(PS[ob][0:M, qoff:512], lhsT=vst, rhs=PTt[:, ptj, qoff:512],
                                                                                              start=(kt == 0), stop=(kt == nkt - 1)),
                     reads=["v", pk], writes=[psk(ob)])
                P.op("pe", lambda e, ptj=ptj, qoff=qoff, kt=kt, db=db: e.matmul(PS[db][:, qoff:512], lhsT=ones_b[:], rhs=PTt[:, ptj, qoff:512],
                                                                                start=(kt == 0), stop=(kt == nkt - 1)),
                     reads=["ones_b", pk], writes=[psk(db)])
                if kt != nkt - 1:
                    if will_yield:
                        yield
                        if i + 1 < len(steps):
                            emit_qk(i + 1)
                    continue
                P.op("act", lambda e, db=db: e.activation(out=rD[:], in_=PS[db][:], func=AF.Ln), reads=[psk(db)], writes=["nrs"])
                P.op("act", lambda e: e.activation(out=rD[:], in_=rD[:], func=AF.Exp, scale=-1.0), reads=["nrs"], writes=["nrs"])
                if mixer == "A":
                    P.op("dve", lambda e, c=c, r0=r0, ob=ob: e.tensor_tensor(out=yT[r0:r0 + 64, c, j * 512:(j + 1) * 512], in0=PS[ob][r0:r0 + 64, :], in1=rD[r0:r0 + 64, :], op=ALU.mult),
                         reads=[psk(ob), "nrs"], writes=["yT"])
                else:
                    hh, comp = u // 2, u % 2
                    if comp == 0:
                        P.op("dve", lambda e, ob=ob: e.tensor_tensor(out=ysb[:, 0, :], in0=PS[ob][:], in1=rD[:], op=ALU.mult), reads=[psk(ob), "nrs"], writes=["ysb0"])
                    else:
                        P.op("dve", lambda e, ob=ob: e.tensor_tensor(out=ysb[:, 1, :], in0=PS[ob][:], in1=rD[:], op=ALU.mult), reads=[psk(ob), "nrs"], writes=["ysb1"])
                        P.op("dve", lambda e: e.scalar_tensor_tensor(out=ysb[:, 0, :], in0=ysb[:, 1, :], scalar=nlam[:, l:l + 1], in1=ysb[:, 0, :], op0=ALU.mult, op1=ALU.add),
                             reads=["ysb0", "ysb1", "nlam"], writes=["ysb0"])
                        P.op("act", lambda e: e.activation(out=sqb[:], in_=ysb[:, 0, :], func=AF.Square), reads=["ysb0"], writes=["sqb"])
                        P.op("pe", lambda e, db=db: e.matmul(PS[db][:, :], lhsT=ones_b[:], rhs=sqb[:], start=True, stop=True), reads=["sqb", "ones_b"], writes=[psk(db)])
                        P.op("act", lambda e, db=db: e.activation(out=rD[:], in_=PS[db][:], func=AF.Ln, scale=1.0 / 128, bias=eps_t[:, 0:1]), reads=[psk(db), "eps"], writes=["nrs"])
                        P.op("act", lambda e: e.activation(out=rD[:], in_=rD[:], func=AF.Exp, scale=-0.5), reads=["nrs"], writes=["nrs"])
                        li = 0.8 - 0.6 * math.exp(-0.3 * l)
                        P.op("dve", lambda e: e.tensor_scalar(out=ysb[:, 1, :], in0=ysb[:, 0, :], scalar1=subln[:, l:l + 1], scalar2=1.0 - li, op0=ALU.mult, op1=ALU.mult),
                             reads=["ysb0", "subln"], writes=["ysb1"])
                        P.op("dve", lambda e, hh=hh: e.tensor_tensor(out=yT[:, hh, j * 512:(j + 1) * 512], in0=ysb[:, 1, :], in1=rD[:], op=ALU.mult),
                             reads=["ysb1", "nrs"], writes=["yT"])
                if will_yield:
                    yield
                    if i + 1 < len(steps):
                        emit_qk(i + 1)

        for b in range(n_seq):
            with contextlib.ExitStack() as ph:
                psb = lambda n, s, d: ph.enter_context(nc.sbuf_tensor(uname(n), s, d))
                pos_i = psb("pos_i", [128, NT], I32)
                pos_f = psb("pos_f", [128, NT], F32)
                ang = psb("ang", [128, NT * 32], F32)
                ki = psb("ki", [128, NT * 32], I32)
                kf = psb("kf", [128, NT * 32], F32)
                rr = psb("rr", [128, NT * 32], F32)
                mm_ = psb("mm_", [128, NT * 32], F32)
                xs = psb("xs", [128, 2, D], F32)
                P.op("sp", lambda e: e.dma_start(out=pos_i[:], in_=pos_d[b]), writes=["pos_i"], dma="pos")
                P.op("dve", lambda e: e.tensor_copy(out=pos_f[:], in_=pos_i[:]), reads=["pos_i"], writes=["pos_f"])
                for t in range(NT):
                    P.op("dve", lambda e, t=t: e.tensor_scalar(out=ang[:, t * 32:(t + 1) * 32], in0=invf, scalar1=pos_f[:, t:t + 1], scalar2=None, op0=ALU.mult),
                         reads=["pos_f", "cf"], writes=["ang"])
                range_sin(ang[:], 0.0, sinT[:].rearrange("p t d -> p (t d)"), ki[:], kf[:], rr[:], mm_[:])
                range_sin(ang[:], math.pi / 2, cosT[:].rearrange("p t d -> p (t d)"), ki[:], kf[:], rr[:], mm_[:])
                for t in range(NT):
                    jx = t % 2
                    P.op("sp", lambda e, t=t, jx=jx: e.dma_start(out=xs[:, jx, :], in_=x_d[b, t * 128:(t + 1) * 128, :]), writes=["xs%d" % jx], dma="xs%d" % jx)
                    for half in range(2):
                        bank = (2 * t + half) % 4
                        for q in range(4):
                            c = half * 4 + q
                            P.op("pe", lambda e, jx=jx, c=c, q=q, bank=bank: e.transpose(out=PS[bank][:, q * 128:(q + 1) * 128], in_=xs[:, jx, c * 128:(c + 1) * 128], identity=ident_f),
                                 reads=["xs%d" % jx, "cf"], writes=[psk(bank)])
                        eng = "act" if half == 0 else "dve"
                        src = PS[bank][:, :].rearrange("p (c t) -> p c t", t=128)
                        dst = xT[:, half * 4:half * 4 + 4, t * 128:(t + 1) * 128]
                        if eng == "act":
                            P.op("act", lambda e, src=src, dst=dst: e.activation(out=dst, in_=src, func=AF.Copy), reads=[psk(bank)], writes=["xT"])
                        else:
                            P.op("dve", lambda e, src=src, dst=dst: e.tensor_copy(out=dst, in_=src), reads=[psk(bank)], writes=["xT"])
                P.barrier()
                P.emit()

            for l in range(depth):
                with contextlib.ExitStack() as ph:
                    psb = lambda n, s, d: ph.enter_context(nc.sbuf_tensor(uname(n), s, d))
                    yaT = psb("yaT", [128, 4, S], BF16)
                    ybT = None
                    for mixer in ("A", "B"):
                        if mixer == "B":
                            ybT = psb("ybT", [128, 4, S], BF16)
                        with contextlib.ExitStack() as ph2:
                            psb2 = lambda n, s, d: ph2.enter_context(nc.sbuf_tensor(uname(n), s, d))
                            kT = psb2("kT", [128, 4, S], BF16)
                            v = psb2("v", [128, NT, 512], BF16)
                            ikT = psb2("ikT", [64, S], BF16)
                            hT = psb2("hT", [128, 8, 512], BF16)
                            sq = psb2("sq", [128, 2, 512], BF16)
                            rs = psb2("rs", [128, 512], F32)
                            scrs = []
                            for si in range(2):
                                scrs.append((psb2("t_sq", [128, 512], BF16), psb2("t_n", [128, 512], F32), psb2("t_a", [128, 512], F32),
                                             psb2("t_b", [128, 512], F32), psb2("ss", [128, 8], F32)))
                            qkb = [psb2("qkb", [128, 512], BF16) for _ in range(2)]
                            ikb = [psb2("ikb", [128, 256], BF16) for _ in range(2)]
                            W1 = psb2("W1", [128, 8, 512], BF16)
                            W3 = psb2("W3", [128, 8, 324], BF16) if mixer == "A" else None
                            kvs = contextlib.ExitStack()
                            W2 = kvs.enter_context(nc.sbuf_tensor(uname("W2"), [128, 8, 512], BF16))
                            yT = yaT if mixer == "A" else ybT
                            gk = hn[:, l, 1, :] if mixer == "A" else hn[:, l, 4, :]
                            gq = hn[:, l, 0, :] if mixer == "A" else hn[:, l, 3, :]
                            ok_, ov_, oq_ = (O_AK, O_AV, O_AQ) if mixer == "A" else (O_BK, O_BV, O_BQ)
                            wload(W1[:], win_view(l, ok_, 512), "W1", "W1")
                            wload(W2[:], win_view(l, ov_, 512), "W2", "W2")
                            if mixer == "A":
                                wload(W3[:], win_view(l, O_IQ, 324), "W3", "W3")
                            def kv_transposes(tile, si):
                                sfx = str(si)
                                for c in range(4):
                                    P.op("pe", lambda e, c=c, si=si: e.transpose(out=PT[0][:, c * 128:(c + 1) * 128], in_=qkb[si][:, c * 128:(c + 1) * 128], identity=ident_b[:]),
                                         reads=["qkb" + sfx, "ident_b"], writes=[ptk(0)])
                                P.op("act", lambda e, tile=tile: e.activation(out=kT[:, :, tile * 128:(tile + 1) * 128], in_=PT[0][:, 0:512].rearrange("p (c t) -> p c t", t=128), func=AF.Copy),
                                     reads=[ptk(0)], writes=["kT"])
                                if mixer == "A":
                                    P.op("pe", lambda e, si=si: e.transpose(out=PT[1][0:64, 0:128], in_=ikb[si][:, 0:64], identity=ident_b[:]), reads=["ikb" + sfx, "ident_b"], writes=[ptk(1)])
                                    P.op("dve", lambda e, tile=tile: e.tensor_copy(out=ikT[:, tile * 128:(tile + 1) * 128], in_=PT[1][0:64, 0:128]), reads=[ptk(1)], writes=["ikT"])

                            kv_pending = None
                            for blk in range(4):
                                norm_block(gattn, l, blk * 512, 512, hT, "hT", sq, rs, "n")
                                for tt in range(4):
                                    tile = blk * 4 + tt
                                    si = tile % 2
                                    sfx = str(si)
                                    kb_, vb_ = tile % 2, 2 + tile % 2
                                    proj_tok(hT, "hT", tt, W1, "W1", 512, kb_)
                                    proj_tok(hT, "hT", tt, W2, "W2", 512, vb_)
                                    P.op("act", lambda e, tile=tile, vb_=vb_: e.activation(out=v[:, tile, :], in_=PS[vb_][:, :], func=AF.Copy), reads=[psk(vb_)], writes=["v"])
                                    postproc(PS[kb_][:, :], psk(kb_), 8, gk, tile, qkb[si][:, :], "qkb" + sfx, scrs[si], sfx=sfx)
                                    if mixer == "A":
                                        proj_tok(hT, "hT", tt, W3, "W3", 324, 4)
                                        postproc(PS[4][:, 256:320], psk(4), 1, hn[:, l, 2, :], tile, ikb[si][:, 0:64], "ikb" + sfx, scrs[si], sfx=sfx)
                                    if kv_pending is not None:
                                        kv_transposes(*kv_pending)
                                    kv_pending = (tile, si)
                            kv_transposes(*kv_pending)
                            P.barrier()
                            P.emit()
                            kvs.close()
                            wload(W1[:], win_view(l, oq_, 512), "W1", "W1")
                            if mixer == "A":
                                qT = psb2("qT", [128, 2, 4, 512], BF16)
                            else:
                                qT = psb2("qT", [128, 2, 2, 4, 512], BF16)
                                P.op("pool", lambda e: e.memset(qT[64:128, :, 0, :, :], 0.0), writes=["qT0", "qT1"])
                                P.op("pool", lambda e: e.memset(qT[0:64, :, 1, :, :], 0.0), writes=["qT0", "qT1"])
                            PTt = psb2("PTt", [128, 3, 512], BF16)
                            rD = rs
                            ysb = psb2("ysb", [128, 2, 512], F32) if mixer == "B" else None
                            sqb = psb2("sqb", [128, 512], BF16) if mixer == "B" else None
                            if mixer == "A":
                                iqT = [psb2("iqT", [64, 4, 128], BF16) for _ in range(2)]
                                iw = [psb2("iw", [128, 12], F32) for _ in range(2)]
                                Sc = [psb2("Sc", [128, S], F32) for _ in range(2)]
                                junk = psb2("junk", [128, S], U8)
                                rl = psb2("rl", [128, 512], F32)
                                mk = psb2("mk", [128, 2, 512], BF16)
                                maskT = psb2("maskT", [128, 2, NT, 512], dt.float8e4)
                                bis = [psb2("bis", [128, NBIS + 8], F32) for _ in range(2)]
                            else:
                                maskT = None

                            def stage1(j):
                                par = j % 2
                                pend_masks = []
                                norm_block(gattn, l, j * 512, 512, hT, "hT", sq, rs, "n", nb=1)
                                yield
                                for tt in range(4):
                                    qi = 4 * j + tt
                                    si = tt % 2
                                    sfx = str(si)
                                    proj_tok(hT, "hT", tt, W1, "W1", 512, 0)
                                    postproc(PS[0][:, :], psk(0), 8, gq, qi, qkb[si][:, :], "qkb" + sfx, scrs[si], sfx=sfx)
                                    nk = 128 * (qi + 1)
                                    if mixer == "A":
                                        Sct, kS, bt, iwt, iqt = Sc[si], "Sc" + sfx, bis[si], iw[si], iqT[si]
                                        thr = bt[:, NBIS + 4:NBIS + 5]
                                        mn, mx, w0 = bt[:, NBIS + 1:NBIS + 2], bt[:, NBIS + 2:NBIS + 3], bt[:, NBIS + 3:NBIS + 4]
                                        mid = bt[:, NBIS + 5:NBIS + 6]
                                        if qi >= 2:
                                            proj_tok(hT, "hT", tt, W3, "W3", 324, 1)
                                            P.op("act", lambda e, iwt=iwt: e.activation(out=iwt[:, 0:4], in_=PS[1][:, 320:324], func=AF.Copy), reads=[psk(1)], writes=["iw" + sfx])
                                            P.op("act", lambda e, iwt=iwt: e.activation(out=iwt[:, 4:8], in_=PS[1][:, 320:324], func=AF.Abs), reads=[psk(1)], writes=["iwa" + sfx])
                                            P.op("dve", lambda e, iwt=iwt: e.tensor_scalar(out=iwt[:, 8:12], in0=iwt[:, 0:4], scalar1=0.0, scalar2=2.0, op0=ALU.is_ge, op1=ALU.mult), reads=["iw" + sfx], writes=["iws" + sfx])
                                            P.op("dve", lambda e, iwt=iwt: e.tensor_scalar(out=iwt[:, 8:12], in0=iwt[:, 8:12], scalar1=-1.0, scalar2=None, op0=ALU.add), reads=["iws" + sfx], writes=["iws" + sfx])
                                            postproc(PS[1][:, 0:256], psk(1), 4, None, qi, ikb[si][:, 0:256], "ikb" + sfx, scrs[si], do_norm=False, sfx=sfx)
                                    yield
                                    for c in range(4):
                                        P.op("pe", lambda e, c=c, si=si: e.transpose(out=PT[0][:, c * 128:(c + 1) * 128], in_=qkb[si][:, c * 128:(c + 1) * 128], identity=ident_b[:]),
                                             reads=["qkb" + sfx, "ident_b"], writes=[ptk(0)])
                                    if mixer == "A":
                                        P.op("act", lambda e, tt=tt, par=par: e.activation(out=qT[:, par, :, tt * 128:(tt + 1) * 128], in_=PT[0][:, 0:512].rearrange("p (c t) -> p c t", t=128), func=AF.Copy),
                                             reads=[ptk(0)], writes=["qT%d" % par])
                                    else:
                                        P.op("act", lambda e, tt=tt, par=par: e.activation(out=qT[0:64, par, 0, :, tt * 128:(tt + 1) * 128], in_=PT[0][0:64, 0:512].rearrange("p (c t) -> p c t", t=128), func=AF.Copy),
                                             reads=[ptk(0)], writes=["qT%d" % par])
                                        P.op("act", lambda e, tt=tt, par=par: e.activation(out=qT[64:128, par, 1, :, tt * 128:(tt + 1) * 128], in_=PT[0][64:128, 0:512].rearrange("p (c t) -> p c t", t=128), func=AF.Copy),
                                             reads=[ptk(0)], writes=["qT%d" % par])
                                    yield
                                    if mixer != "A":
                                        continue
                                    if tt == 2 and pend_masks:
                                        yield from pend_masks.pop()
                                    if qi < 2:
                                        P.op("dve", lambda e, nk=nk, Sct=Sct: e.memset(Sct[:, 0:nk], 0.0), writes=[kS])
                                        P.op("dve", lambda e, qi=qi, Sct=Sct: e.tensor_copy(out=Sct[:, qi * 128:(qi + 1) * 128], in_=caus_b), reads=["cf"], writes=[kS])
                                        P.op("dve", lambda e, thr=thr: e.memset(thr, -1e29), writes=["thr" + sfx])
                                    else:
                                        for h in range(4):
                                            P.op("pe", lambda e, h=h, si=si: e.transpose(out=PT[1][0:64, h * 128:(h + 1) * 128], in_=ikb[si][:, h * 64:(h + 1) * 64], identity=ident_b[:]),
                                                 reads=["ikb" + sfx, "ident_b"], writes=[ptk(1)])
                                        P.op("dve", lambda e, iqt=iqt: e.tensor_copy(out=iqt[:, :, :], in_=PT[1][0:64, 0:512].rearrange("p (h t) -> p h t", t=128)), reads=[ptk(1)], writes=["iqT" + sfx])
                                        yield
                                        for kb in range((nk + 511) // 512):
                                            w = min(512, nk - kb * 512)
                                            for h in range(4):
                                                bank = h % 2
                                                P.op("pe", lambda e, h=h, kb=kb, w=w, bank=bank, iqt=iqt: e.matmul(PS[bank][:, 0:w], lhsT=iqt[:, h, :], rhs=ikT[:, kb * 512:kb * 512 + w], start=True, stop=True),
                                                     reads=["iqT" + sfx, "ikT"], writes=[psk(bank)])
                                                P.op("act", lambda e, h=h, w=w, bank=bank, iwt=iwt: e.activation(out=rl[:, 0:w], in_=PS[bank][:, 0:w], func=AF.Relu, scale=iwt[:, 4 + h:5 + h]),
                                                     reads=[psk(bank), "iwa" + sfx], writes=["rl"])
                                                if h == 0:
                                                    P.op("dve", lambda e, kb=kb, w=w, Sct=Sct, iwt=iwt: e.tensor_scalar(out=Sct[:, kb * 512:kb * 512 + w], in0=rl[:, 0:w], scalar1=iwt[:, 8:9], scalar2=None, op0=ALU.mult),
                                                         reads=["rl", "iws" + sfx], writes=[kS])
                                                else:
                                                    P.op("dve", lambda e, h=h, kb=kb, w=w, Sct=Sct, iwt=iwt: e.scalar_tensor_tensor(out=Sct[:, kb * 512:kb * 512 + w], in0=rl[:, 0:w], scalar=iwt[:, 8 + h:9 + h],
                                                                                                                            in1=Sct[:, kb * 512:kb * 512 + w], op0=ALU.mult, op1=ALU.add),
                                                         reads=["rl", "iws" + sfx, kS], writes=[kS])
                                            yield
                                        P.op("dve", lambda e, nk=nk, Sct=Sct, mn=mn: e.tensor_reduce(out=mn, in_=Sct[:, 0:nk], axis=AX.X, op=ALU.min), reads=[kS], writes=["mn" + sfx])
                                        P.op("dve", lambda e, qi=qi, Sct=Sct: e.tensor_tensor(out=Sct[:, qi * 128:(qi + 1) * 128], in0=Sct[:, qi * 128:(qi + 1) * 128], in1=caus_b, op=ALU.add),
                                             reads=[kS, "cf"], writes=[kS])
                                        P.op("dve", lambda e, nk=nk, Sct=Sct, mx=mx: e.tensor_reduce(out=mx, in_=Sct[:, 0:nk], axis=AX.X, op=ALU.max), reads=[kS], writes=["mx" + sfx])
                                        P.op("dve", lambda e, mx=mx, mn=mn, w0=w0: e.tensor_tensor(out=w0, in0=mx, in1=mn, op=ALU.subtract), reads=["mx" + sfx, "mn" + sfx], writes=["w0" + sfx])
                                        P.op("dve", lambda e, bt=bt, w0=w0: e.tensor_scalar(out=bt[:, 0:NBIS + 1], in0=pow2, scalar1=w0, scalar2=None, op0=ALU.mult), reads=["w0" + sfx, "cf"], writes=["hk" + sfx])
                                        P.op("dve", lambda e, bt=bt, mn=mn, mid=mid: e.tensor_tensor(out=mid, in0=mn, in1=bt[:, 0:1], op=ALU.add), reads=["mn" + sfx, "hk" + sfx], writes=["mid" + sfx])
                                        yield
                                    if tt % 2 == 0:
                                        continue
                                    pair = [t for t in (tt - 1, tt) if 4 * j + t >= 2]
                                    for k in range(NBIS):
                                        for t in pair:
                                            f_ = str(t % 2)
                                            bt2, nk2 = bis[t % 2], 128 * (4 * j + t + 1)
                                            if t % 2 == 0 or not ACT_COUNT:
                                                P.op("dve", lambda e, bt2=bt2, nk2=nk2, t=t: e.tensor_scalar(out=junk[:, 0:nk2], in0=Sc[t % 2][:, 0:nk2], scalar1=bt2[:, NBIS + 5:NBIS + 6], scalar2=None,
                                                                                                         op0=ALU.is_ge, op1=ALU.add, accum_out=bt2[:, NBIS + 7:NBIS + 8]),
                                                     reads=["Sc" + f_, "mid" + f_], writes=["cnt" + f_])
                                            else:
                                                P.op("act", lambda e, bt2=bt2, nk2=nk2, t=t: e.activation(out=rl[:, :].bitcast(U8)[:, 0:nk2], in_=Sc[t % 2][:, 0:nk2], func=AF.Sign, scale=-1.0,
                                                                                                      bias=bt2[:, NBIS + 5:NBIS + 6], accum_out=bt2[:, NBIS + 7:NBIS + 8]),
                                                     reads=["Sc" + f_, "mid" + f_], writes=["cnt" + f_, "rl"])
                                        for t in pair:
                                            f_ = str(t % 2)
                                            bt2, nk2 = bis[t % 2], 128 * (4 * j + t + 1)
                                            if t % 2 == 0 or not ACT_COUNT:
                                                P.op("dve", lambda e, bt2=bt2, k=k: e.tensor_scalar(out=bt2[:, NBIS + 6:NBIS + 7], in0=bt2[:, NBIS + 7:NBIS + 8], scalar1=256.0, scalar2=bt2[:, k:k + 1], op0=ALU.is_ge, op1=ALU.mult),
                                                     reads=["cnt" + f_, "hk" + f_], writes=["tq" + f_])
                                            else:
                                                P.op("dve", lambda e, bt2=bt2, k=k, nk2=nk2: e.tensor_scalar(out=bt2[:, NBIS + 6:NBIS + 7], in0=bt2[:, NBIS + 7:NBIS + 8], scalar1=float(nk2 - 512), scalar2=bt2[:, k:k + 1], op0=ALU.is_le, op1=ALU.mult),
                                                     reads=["cnt" + f_, "hk" + f_], writes=["tq" + f_])
                                        for t in pair:
                                            f_ = str(t % 2)
                                            bt2 = bis[t % 2]
                                            P.op("dve", lambda e, bt2=bt2, k=k: e.scalar_tensor_tensor(out=bt2[:, NBIS + 5:NBIS + 6], in0=bt2[:, NBIS + 6:NBIS + 7], scalar=bt2[:, k + 1:k + 2], in1=bt2[:, NBIS + 5:NBIS + 6],
                                                                                                     op0=ALU.subtract, op1=ALU.add),
                                                 reads=["tq" + f_, "hk" + f_, "mid" + f_], writes=["mid" + f_])
                                        if pair:
                                            yield
                                    for t in pair:
                                        f_ = str(t % 2)
                                        bt2 = bis[t % 2]
                                        P.op("dve", lambda e, bt2=bt2: e.tensor_tensor(out=bt2[:, NBIS + 4:NBIS + 5], in0=bt2[:, NBIS + 5:NBIS + 6], in1=bt2[:, NBIS:NBIS + 1], op=ALU.subtract),
                                             reads=["mid" + f_, "hk" + f_], writes=["thr" + f_])

                                    def emit_masks(tiles, j=j, par=par):
                                        for t in tiles:
                                            f_ = str(t % 2)
                                            bt2, nk2 = bis[t % 2], 128 * (4 * j + t + 1)
                                            for kb in range((nk2 + 511) // 512):
                                                w = min(512, nk2 - kb * 512)
                                                mj = kb % 2
                                                P.op("dve", lambda e, kb=kb, w=w, mj=mj, t=t, bt2=bt2: e.tensor_scalar(out=mk[:, mj, 0:w], in0=Sc[t % 2][:, kb * 512:kb * 512 + w], scalar1=bt2[:, NBIS + 4:NBIS + 5], scalar2=None, op0=ALU.is_lt),
                                                     reads=["Sc" + f_, "thr" + f_], writes=["mk%d" % mj])
                                                nt_ = w // 128
                                                for q in range(nt_):
                                                    P.op("pe", lambda e, q=q, mj=mj: e.transpose(out=PT[1][:, q * 128:(q + 1) * 128], in_=mk[:, mj, q * 128:(q + 1) * 128], identity=ident_b[:]),
                                                         reads=["mk%d" % mj, "ident_b"], writes=[ptk(1)])
                                                P.op("act", lambda e, kb=kb, nt_=nt_, t=t, par=par: e.activation(out=maskT[:, par, kb * 4:kb * 4 + nt_, t * 128:(t + 1) * 128],
                                                                                                       in_=PT[1][:, 0:nt_ * 128].rearrange("p (c t) -> p c t", t=128), func=AF.Copy, saturate=False),
                                                     reads=[ptk(1)], writes=["maskT%d" % par])
                                                yield

                                    if tt == 1:
                                        pend_masks.append(emit_masks((0, 1)))
                                    else:
                                        yield from emit_masks((2, 3))

                            def att_gen(j):
                                par = j % 2
                                mT_ = maskT[:, par] if mixer == "A" else None
                                yield from attention(mixer, l, j, kT, v, qT[:, par], mT_, yT, PTt, rD, ysb, sqb, ratio=(4 if mixer == "A" else 8))

                            for _ in stage1(0):
                                pass
                            for j in range(4):
                                ga = att_gen(j)
                                gs = stage1(j + 1) if j < 3 else iter(())
                                n_att = (8 * (4 * j + 4)) // (4 if mixer == "A" else 8)
                                n_s1 = 100 if mixer == "A" else 5
                                kadv = max(1, -(-n_s1 // max(1, n_att - 1)))
                                for _ in ga:
                                    for _k in range(kadv):
                                        next(gs, None)
                                for _ in gs:
                                    pass
                            if dbg and b == 0 and l == 0:
                                dump("dbg_y" + mixer, yT[:], [128, 4, S], BF16, ["yT"])
                            P.barrier()
                            P.emit()

                    with contextlib.ExitStack() as ph2:
                        psb2 = lambda n, s, d: ph2.enter_context(nc.sbuf_tensor(uname(n), s, d))
                        hT = psb2("hTm", [128, 8, 1024], BF16)
                        mT = psb2("mT", [128, 8, 1024], BF16)
                        sq = psb2("sqm", [128, 2, 512], BF16)
                        rs = psb2("rsm", [128, 512], F32)
                        Wg = psb2("Wg", [128, 2, 8, 256], BF16)
                        Wu = psb2("Wu", [128, 2, 2, 4, 128], BF16)
                        Wo = psb2("Wo", [128, 2, 8, 128], BF16)
                        sa = psb2("sa", [128, 2, 512], F32)
                        m1 = psb2("m1", [128, 2, 512], F32)
                        for half in range(2):
                            for bb in range(2):
                                norm_block(gattn, l, half * 1024 + bb * 512, 512, hT[:, :, bb * 512:(bb + 1) * 512], "hTm", sq, rs, "m")
                            for dc in range(8):
                                wj = dc % 2
                                for gi in range(2):
                                    wload(Wg[:, wj, :, gi * 128:(gi + 1) * 128], win_view(l, O_G + gi * 1024 + dc * 128, 128), "Wg%d" % wj, "Wg%d" % wj)
                                wload(Wu[:, wj, 0, :, :], wua_d[l].rearrange("(kc p) n -> p kc n", p=128)[:, :, dc * 128:(dc + 1) * 128], "Wu%d" % wj, "Wu%d" % wj)
                                wload(Wu[:, wj, 1, :, :], wub_d[l].rearrange("(kc p) n -> p kc n", p=128)[:, :, dc * 128:(dc + 1) * 128], "Wu%d" % wj, "Wu%d" % wj)
                                for bb in range(2):
                                    t0 = half * 1024 + bb * 512
                                    for gi in range(2):
                                        for kc in range(8):
                                            P.op("pe", lambda e, gi=gi, kc=kc, wj=wj, bb=bb: e.matmul(PS[gi][:, :], lhsT=Wg[:, wj, kc, gi * 128:(gi + 1) * 128], rhs=hT[:, kc, bb * 512:(bb + 1) * 512],
                                                                                                   start=(kc == 0), stop=(kc == 7)),
                                                 reads=["hTm", "Wg%d" % wj], writes=[psk(gi)])
                                        yy = yaT if gi == 0 else ybT
                                        for kc in range(4):
                                            P.op("pe", lambda e, gi=gi, kc=kc, wj=wj, yy=yy, t0=t0: e.matmul(PS[2 + gi][:, :], lhsT=Wu[:, wj, gi, kc, :], rhs=yy[:, kc, t0:t0 + 512],
                                                                                                         start=(kc == 0), stop=(kc == 3)),
                                                 reads=["yaT" if gi == 0 else "ybT", "Wu%d" % wj], writes=[psk(2 + gi)])
                                        P.op("act", lambda e, gi=gi, dc=dc: e.activation(out=sa[:, gi, :], in_=PS[gi][:, :], func=AF.Sigmoid, bias=gbias[:, l, gi * 8 + dc:gi * 8 + dc + 1]),
                                             reads=[psk(gi), "gbias"], writes=["sa%d" % gi])
                                        P.op("dve", lambda e, gi=gi: e.tensor_tensor(out=m1[:, gi, :], in0=sa[:, gi, :], in1=PS[2 + gi][:, :], op=ALU.mult),
                                             reads=["sa%d" % gi, psk(2 + gi)], writes=["m1%d" % gi])
                                    P.op("dve", lambda e, dc=dc, bb=bb: e.tensor_tensor(out=mT[:, dc, bb * 512:(bb + 1) * 512], in0=m1[:, 0, :], in1=m1[:, 1, :], op=ALU.add),
                                         reads=["m10", "m11"], writes=["mT"])
                            for dc in range(8):
                                wj = dc % 2
                                wload(Wo[:, wj, :, :], wo_d[l].rearrange("(kc p) n -> p kc n", p=128)[:, :, dc * 128:(dc + 1) * 128], "Wo%d" % wj, "Wo%d" % wj)
                                for bb in range(2):
                                    t0 = half * 1024 + bb * 512
                                    bank = 4 + bb
                                    for kc in range(8):
                                        P.op("pe", lambda e, kc=kc, wj=wj, bb=bb, bank=bank: e.matmul(PS[bank][:, :], lhsT=Wo[:, wj, kc, :], rhs=mT[:, kc, bb * 512:(bb + 1) * 512],
                                                                                                  start=(kc == 0), stop=(kc == 7)),
                                             reads=["mT", "Wo%d" % wj], writes=[psk(bank)])
                                    P.op("dve", lambda e, dc=dc, t0=t0, bank=bank: e.tensor_tensor(out=xT[:, dc, t0:t0 + 512], in0=xT[:, dc, t0:t0 + 512], in1=PS[bank][:, :], op=ALU.add),
                                         reads=["xT", psk(bank)], writes=["xT"])
                            P.barrier()
                        if b == 0 and l == 0:
                            dump("dbg_x1", xT[:], [128, 8, S], F32, ["xT"])
                            P.barrier()
                        P.emit()

                with contextlib.ExitStack() as ph:
                    psb = lambda n, s, d: ph.enter_context(nc.sbuf_tensor(uname(n), s, d))
                    h2 = psb("h2", [128, 8, S], BF16)
                    sq = psb("sqf", [128, 2, 512], BF16)
                    rs = psb("rsf", [128, 512], F32)
                    Wfi = psb("Wfi", [128, 2, 8, 2, 512], BF16)
                    Wfo = psb("Wfo", [128, 2, 4, D], BF16)
                    sg = psb("sg", [128, 2, 512], F32)
                    act = psb("actf", [128, 4, 512], BF16)
                    for blk in range(4):
                        norm_block(gffn, l, blk * 512, 512, h2[:, :, blk * 512:(blk + 1) * 512], "h2", sq, rs, "f")
                    groups = [(g0, min(4, 22 - g0)) for g0 in range(0, 22, 4)]
                    for gi, (g0, G) in enumerate(groups):
                        wj = gi % 2
                        for gu in range(2):
                            wload(Wfi[:, wj, :, gu, 0:G * 128], wfi_d[l].rearrange("(kc p) n -> p kc n", p=128)[:, :, gu * FH + g0 * 128:gu * FH + (g0 + G) * 128], "Wfi%d" % wj, "Wfi%d" % wj)
                        wload(Wfo[:, wj, 0:G, :], wfo_d[l][g0 * 128:(g0 + G) * 128, :].rearrange("(g p) n -> p g n", p=128), "Wfo%d" % wj, "Wfo%d" % wj)
                        for blk in range(4):
                            for g in range(G):
                                for gu in range(2):
                                    for kc in range(8):
                                        P.op("pe", lambda e, g=g, gu=gu, kc=kc, wj=wj, blk=blk: e.matmul(PS[gu][:, :], lhsT=Wfi[:, wj, kc, gu, g * 128:(g + 1) * 128], rhs=h2[:, kc, blk * 512:(blk + 1) * 512],
                                                                                                     start=(kc == 0), stop=(kc == 7)),
                                             reads=["h2", "Wfi%d" % wj], writes=[psk(gu)])
                                sj = g % 2
                                P.op("act", lambda e, sj=sj: e.activation(out=sg[:, sj, :], in_=PS[0][:, :], func=AF.Silu), reads=[psk(0)], writes=["sg%d" % sj])
                                P.op("dve", lambda e, sj=sj, g=g: e.tensor_tensor(out=act[:, g, :], in0=sg[:, sj, :], in1=PS[1][:, :], op=ALU.mult),
                                     reads=["sg%d" % sj, psk(1)], writes=["act%d" % g])
                            for dc in range(8):
                                bank = 2 + dc % 4
                                for g in range(G):
                                    P.op("pe", lambda e, g=g, dc=dc, wj=wj, bank=bank: e.matmul(PS[bank][:, :], lhsT=Wfo[:, wj, g, dc * 128:(dc + 1) * 128], rhs=act[:, g, :],
                                                                                            start=(g == 0), stop=(g == G - 1)),
                                         reads=["act%d" % g, "Wfo%d" % wj], writes=[psk(bank)])
                                P.op("dve", lambda e, dc=dc, blk=blk, bank=bank: e.tensor_tensor(out=xT[:, dc, blk * 512:(blk + 1) * 512], in0=xT[:, dc, blk * 512:(blk + 1) * 512], in1=PS[bank][:, :], op=ALU.add),
                                     reads=["xT", psk(bank)], writes=["xT"])
                    P.barrier()
                    P.emit()

            with contextlib.ExitStack() as ph:
                psb = lambda n, s, d: ph.enter_context(nc.sbuf_tensor(uname(n), s, d))
                xo = psb("xo", [128, 2, D], F32)
                for t in range(NT):
                    jx = t % 2
                    for half in range(2):
                        bank = (2 * t + half) % 4
                        for q in range(4):
                            c = half * 4 + q
                            P.op("pe", lambda e, t=t, c=c, q=q, bank=bank: e.transpose(out=PS[bank][:, q * 128:(q + 1) * 128], in_=xT[:, c, t * 128:(t + 1) * 128], identity=ident_f),
                                 reads=["xT", "cf"], writes=[psk(bank)])
                        if half == 0:
                            P.op("act", lambda e, jx=jx, bank=bank: e.activation(out=xo[:, jx, 0:512], in_=PS[bank][:, :], func=AF.Copy), reads=[psk(bank)], writes=["xo%d" % jx])
                        else:
                            P.op("dve", lambda e, jx=jx, bank=bank: e.tensor_copy(out=xo[:, jx, 512:1024], in_=PS[bank][:, :]), reads=[psk(bank)], writes=["xo%d" % jx])
                    o = P.op("sp", lambda e, t=t, jx=jx: e.dma_start(out=out_d[b, t * 128:(t + 1) * 128, :], in_=xo[:, jx, :]), reads=["xo%d" % jx], dma="xo%d" % jx)
                    P.final.append(o)
                P.barrier()
                P.emit(final=(b == n_seq - 1))
    return nc


def host_consts(depth, inputs):
    cfa = np.zeros((128, 128 * 3 + 32 + NBIS + 1), np.float32)
    cfa[:, 0:128] = np.eye(128, dtype=np.float32)
    qq, kk = np.meshgrid(np.arange(128), np.arange(128), indexing="ij")
    cfa[:, 128:256] = np.where(kk <= qq, 0.0, -1e30)
    cfa[:, 256:384] = (qq <= kk).astype(np.float32)
    cfa[:, 384:416] = (1.0 / (10000.0 ** (np.arange(0, 64, 2, dtype=np.float32) / 64.0))).astype(np.float32)[None, :]
    cfa[:, 416:416 + NBIS + 1] = (0.5 ** np.arange(1, NBIS + 2)).astype(np.float32)[None, :]
    fm = lambda a, nch: np.ascontiguousarray(np.asarray(a, np.float32).reshape(depth, nch, 128).transpose(2, 0, 1))
    rep = lambda a: np.ascontiguousarray(np.broadcast_to(np.asarray(a, np.float32)[None], (128,) + np.asarray(a).shape))
    hnorm = np.stack([inputs[k] for k in ("a_q_norm", "a_k_norm", "idx_k_norm", "b_q_norm", "b_k_norm")], axis=1)
    return {
        "cf32": cfa,
        "gattn": fm(inputs["attn_norm"], 8),
        "gffn": fm(inputs["ffn_norm"], 8),
        "gbias": fm(inputs["gate_bias"], 16),
        "hnorm": rep(hnorm),
        "subln": np.ascontiguousarray(np.asarray(inputs["b_subln"], np.float32).T),
        "dlam": rep(inputs["diff_lambda"]),
    }


def run(inputs, n_cores=8, n_seq=4, depth=2, dbg=False):
    inputs = {k: np.asarray(v) for k, v in inputs.items()}
    inputs = {k: (v if k in ("x", "positions") else v[:depth]) for k, v in inputs.items()}
    nc = build(n_seq, depth, dbg)
    common = host_consts(depth, inputs)
    for k in ("w_in", "w_up_a", "w_up_b", "w_out", "w_ffn_in", "w_ffn_out"):
        common[k] = np.ascontiguousarray(inputs[k][:depth], dtype=np.float32)
    in_maps = []
    for c in range(n_cores):
        m = dict(common)
        m["x"] = np.ascontiguousarray(inputs["x"][c * n_seq:(c + 1) * n_seq], dtype=np.float32)
        p = np.asarray(inputs["positions"][c * n_seq:(c + 1) * n_seq], np.int32)
        m["pos"] = np.ascontiguousarray(p.reshape(n_seq, NT, 128).transpose(0, 2, 1))
        in_maps.append(m)
    res = run_bass_kernel_spmd(nc, in_maps, core_ids=list(range(n_cores)))
    if dbg:
        return res.results
    return np.concatenate([r["out"] for r in res.results], axis=0)


def kernel(**inputs):
    return run(inputs, 8, 4, 2).astype(np.float32)
```
